# Optimizing a Trainium2 kernel written in Bass

```python
import jax, jax.numpy as jnp
from jax import lax
import numpy as np


D_MODEL = 1024
BATCH = 32
SEQ = 2048
DEPTH = 2

N_A_LAYERS = DEPTH // 2
N_B_LAYERS = DEPTH - N_A_LAYERS
HEAD_DIM = 64
MIX_WIDTH = D_MODEL
MEM_TOKENS = 256
MEM_HEADS = 4
MEM_WIDTH = MEM_HEADS * HEAD_DIM
POOL_WIDTH = MIX_WIDTH - MEM_WIDTH
POOL_WINDOWS = (2, 4, 8, 16)
POOL_GROUPS = len(POOL_WINDOWS)
POOL_GROUP_DIM = POOL_WIDTH // POOL_GROUPS
FOX_WIDTH = MIX_WIDTH - MEM_WIDTH
FOX_HEADS = FOX_WIDTH // HEAD_DIM
KV_SHARED_WIDTH = 2 * FOX_WIDTH + FOX_HEADS
Q_BLOCK = 128
N_EXPERTS = 32
TOP_K = 4
D_FF = D_MODEL
SWIGLU_ALPHA = 1.702
SWIGLU_LIMIT = 7.0
MOE_BLOCK = 512
DN_ALPHA = float((2 * DEPTH) ** 0.25)
DN_BETA = float((8 * DEPTH) ** -0.25)
LN_EPS = 1e-5
ATTN_SCALE = HEAD_DIM ** -0.5

kernel_name = 'hybrid_pool_fox_moe_deepnorm'


def layer_norm(x, g, b):
    xf = x.astype(jnp.float32)
    mu = jnp.mean(xf, axis=-1, keepdims=True)
    var = jnp.mean(jnp.square(xf - mu), axis=-1, keepdims=True)
    y = (xf - mu) * lax.rsqrt(var + LN_EPS) * g.astype(jnp.float32) + b.astype(jnp.float32)
    return y.astype(x.dtype)


def multi_scale_pool(u, pool_w, pool_scale):
    B, S, _ = u.shape
    ug = u.reshape(B, S, POOL_GROUPS, POOL_GROUP_DIM).astype(jnp.float32)
    cs = jnp.cumsum(ug, axis=1)
    t = jnp.arange(S)
    outs = []
    for g, w in enumerate(POOL_WINDOWS):
        c = cs[:, :, g]
        prev = jnp.pad(c, ((0, 0), (w, 0), (0, 0)))[:, :S]
        cnt = jnp.minimum(t + 1, w).astype(jnp.float32)[None, :, None]
        outs.append((c - prev) / cnt - ug[:, :, g])
    d = jnp.stack(outs, axis=2).astype(u.dtype)
    y = jnp.einsum('bsgc,gcd->bsgd', d, pool_w).reshape(B, S, POOL_WIDTH)
    return y * pool_scale


def memory_attention(q_flat, mem, w_mem_kv):
    B, S, _ = q_flat.shape
    M = mem.shape[1]
    q = q_flat.reshape(B, S, MEM_HEADS, HEAD_DIM)
    kv = mem @ w_mem_kv
    mk = kv[..., :MEM_WIDTH].reshape(B, M, MEM_HEADS, HEAD_DIM)
    mv = kv[..., MEM_WIDTH:].reshape(B, M, MEM_HEADS, HEAD_DIM)
    s = jnp.einsum('bshd,bmhd->bhsm', q, mk).astype(jnp.float32) * ATTN_SCALE
    p = jax.nn.softmax(s, axis=-1).astype(mv.dtype)
    o = jnp.einsum('bhsm,bmhd->bshd', p, mv)
    return o.reshape(B, S, MEM_WIDTH)


def shared_kv(h, kv_w, fgate_b):
    B, S, _ = h.shape
    proj = h @ kv_w
    k = proj[..., :FOX_WIDTH].reshape(B, S, FOX_HEADS, HEAD_DIM).transpose(0, 2, 1, 3)
    v = proj[..., FOX_WIDTH:2 * FOX_WIDTH].reshape(B, S, FOX_HEADS, HEAD_DIM).transpose(0, 2, 1, 3)
    logf = jax.nn.log_sigmoid((proj[..., 2 * FOX_WIDTH:] + fgate_b).astype(jnp.float32))
    c = jnp.cumsum(logf, axis=1).transpose(0, 2, 1)
    return k, v, c


def forgetting_attention(q_flat, k, v, c):
    B, S, _ = q_flat.shape
    q = q_flat.reshape(B, S, FOX_HEADS, HEAD_DIM).transpose(0, 2, 1, 3)
    outs = []
    for i in range(S // Q_BLOCK):
        q0 = i * Q_BLOCK
        q1 = q0 + Q_BLOCK
        s = jnp.einsum('bhqd,bhkd->bhqk', q[:, :, q0:q1], k[:, :, :q1]).astype(jnp.float32) * ATTN_SCALE
        s = s + c[:, :, q0:q1, None] - c[:, :, None, :q1]
        causal = jnp.arange(q1)[None, :] <= jnp.arange(q0, q1)[:, None]
        s = jnp.where(causal, s, -jnp.inf)
        p = jax.nn.softmax(s, axis=-1).astype(v.dtype)
        outs.append(jnp.einsum('bhqk,bhkd->bhqd', p, v[:, :, :q1]))
    o = jnp.concatenate(outs, axis=2)
    return o.transpose(0, 2, 1, 3).reshape(B, S, FOX_WIDTH)


def clamped_swiglu(hdn):
    x_glu = jnp.minimum(hdn[..., ::2], SWIGLU_LIMIT)
    x_lin = jnp.clip(hdn[..., 1::2], -SWIGLU_LIMIT, SWIGLU_LIMIT)
    return x_glu * jax.nn.sigmoid(SWIGLU_ALPHA * x_glu) * (x_lin + 1.0)


def moe(x, router_w, router_b, w1, b1, w2, b2):
    B, S, D = x.shape
    T = B * S
    xt = x.reshape(T, D)
    logits = (xt @ router_w + router_b).astype(jnp.float32)
    top_vals, top_idx = lax.top_k(logits, TOP_K)
    gates = jax.nn.softmax(top_vals, axis=-1).astype(x.dtype)
    n_assign = T * TOP_K
    e_flat = top_idx.reshape(-1).astype(jnp.int32)
    tok_flat = jnp.arange(n_assign, dtype=jnp.int32) // TOP_K
    g_flat = gates.reshape(-1)
    order = jnp.argsort(e_flat)
    e_sorted = e_flat[order]
    tok_sorted = tok_flat[order]
    g_sorted = g_flat[order]
    counts = jnp.bincount(e_flat, length=N_EXPERTS)
    padded = ((counts + MOE_BLOCK - 1) // MOE_BLOCK) * MOE_BLOCK
    start = jnp.cumsum(counts) - counts
    pad_end = jnp.cumsum(padded)
    pad_start = pad_end - padded
    dest = pad_start[e_sorted] + (jnp.arange(n_assign, dtype=jnp.int32) - start[e_sorted])
    n_blocks = -(-n_assign // MOE_BLOCK) + N_EXPERTS
    n_slots = n_blocks * MOE_BLOCK
    slot_tok = jnp.full((n_slots,), T, dtype=jnp.int32).at[dest].set(tok_sorted)
    slot_gate = jnp.zeros((n_slots,), x.dtype).at[dest].set(g_sorted)
    block_expert = jnp.clip(jnp.searchsorted(pad_end, jnp.arange(n_blocks) * MOE_BLOCK, side='right'),
                            0, N_EXPERTS - 1)
    xpad = jnp.concatenate([xt, jnp.zeros((1, D), x.dtype)], axis=0)

    def step(acc, blk):
        e, tok, g = blk
        hdn = xpad[tok] @ w1[e] + b1[e]
        y = (clamped_swiglu(hdn) @ w2[e] + b2[e]) * g[:, None]
        return acc.at[tok].add(y), None

    acc0 = jnp.zeros((T + 1, D), x.dtype)
    acc, _ = lax.scan(step, acc0, (block_expert, slot_tok.reshape(n_blocks, MOE_BLOCK),
                                   slot_gate.reshape(n_blocks, MOE_BLOCK)))
    return acc[:T].reshape(B, S, D)


def setup_inputs(seed: int = 0) -> dict:
    key = jax.random.key(seed)
    ks = jax.random.split(key, 24)

    def nrm(k, shape, scale):
        return jax.random.normal(k, shape, jnp.float32) * scale

    D, E, F = D_MODEL, N_EXPERTS, D_FF
    kv_cols = jnp.concatenate([jnp.ones((FOX_WIDTH,)), jnp.full((FOX_WIDTH,), DN_BETA),
                               jnp.ones((FOX_HEADS,))]).astype(jnp.float32)
    mem_cols = jnp.concatenate([jnp.ones((MEM_WIDTH,)), jnp.full((MEM_WIDTH,), DN_BETA)]).astype(jnp.float32)
    return {
        'x': nrm(ks[0], (BATCH, SEQ, D), 1.0),
        'mem': nrm(ks[1], (BATCH, MEM_TOKENS, D), 1.0),
        'ln1_g': 1.0 + nrm(ks[2], (DEPTH, D), 0.02),
        'ln1_b': nrm(ks[3], (DEPTH, D), 0.02),
        'ln2_g': 1.0 + nrm(ks[4], (DEPTH, D), 0.02),
        'ln2_b': nrm(ks[5], (DEPTH, D), 0.02),
        'a_w_in': nrm(ks[6], (N_A_LAYERS, D, MIX_WIDTH), D ** -0.5),
        'pool_w': nrm(ks[7], (N_A_LAYERS, POOL_GROUPS, POOL_GROUP_DIM, POOL_GROUP_DIM), POOL_GROUP_DIM ** -0.5),
        'pool_scale': 1.0 + nrm(ks[8], (N_A_LAYERS, POOL_WIDTH), 0.1),
        'a_mem_kv': nrm(ks[9], (N_A_LAYERS, D, 2 * MEM_WIDTH), D ** -0.5) * mem_cols,
        'a_w_out': nrm(ks[10], (N_A_LAYERS, MIX_WIDTH, D), MIX_WIDTH ** -0.5 * DN_BETA),
        'kv_w': nrm(ks[11], (D, KV_SHARED_WIDTH), D ** -0.5) * kv_cols,
        'fgate_b': jax.random.uniform(ks[12], (FOX_HEADS,), jnp.float32, 1.0, 4.0),
        'b_w_in': nrm(ks[13], (N_B_LAYERS, D, MIX_WIDTH), D ** -0.5),
        'b_mem_kv': nrm(ks[14], (N_B_LAYERS, D, 2 * MEM_WIDTH), D ** -0.5) * mem_cols,
        'b_w_out': nrm(ks[15], (N_B_LAYERS, MIX_WIDTH, D), MIX_WIDTH ** -0.5 * DN_BETA),
        'router_w': nrm(ks[16], (DEPTH, D, E), D ** -0.5),
        'router_b': nrm(ks[17], (DEPTH, E), 0.01),
        'moe_w1': nrm(ks[18], (DEPTH, E, D, 2 * F), D ** -0.5 * DN_BETA),
        'moe_b1': nrm(ks[19], (DEPTH, E, 2 * F), 0.01),
        'moe_w2': nrm(ks[20], (DEPTH, E, F, D), F ** -0.5 * DN_BETA),
        'moe_b2': nrm(ks[21], (DEPTH, E, D), 0.01),
    }


def reference(x, mem, ln1_g, ln1_b, ln2_g, ln2_b, a_w_in, pool_w, pool_scale, a_mem_kv, a_w_out,
              kv_w, fgate_b, b_w_in, b_mem_kv, b_w_out, router_w, router_b, moe_w1, moe_b1, moe_w2, moe_b2):
    h = x
    k_sh = v_sh = c_sh = None
    for l in range(DEPTH):
        if l < N_A_LAYERS:
            proj = h @ a_w_in[l]
            pooled = multi_scale_pool(proj[..., :POOL_WIDTH], pool_w[l], pool_scale[l])
            memo = memory_attention(proj[..., POOL_WIDTH:], mem, a_mem_kv[l])
            mix = jnp.concatenate([pooled, memo], axis=-1) @ a_w_out[l]
        else:
            j = l - N_A_LAYERS
            if j == 0:
                k_sh, v_sh, c_sh = shared_kv(h, kv_w, fgate_b)
            proj = h @ b_w_in[j]
            fox = forgetting_attention(proj[..., :FOX_WIDTH], k_sh, v_sh, c_sh)
            memo = memory_attention(proj[..., FOX_WIDTH:], mem, b_mem_kv[j])
            mix = jnp.concatenate([fox, memo], axis=-1) @ b_w_out[j]
        h = layer_norm(DN_ALPHA * h + mix, ln1_g[l], ln1_b[l])
        ffn = moe(h, router_w[l], router_b[l], moe_w1[l], moe_b1[l], moe_w2[l], moe_b2[l])
        h = layer_norm(DN_ALPHA * h + ffn, ln2_g[l], ln2_b[l])
    return h
```

```python
import numpy as np
from contextlib import ExitStack
import concourse.bass as bass
import concourse.mybir as mybir
from concourse.bass_utils import run_bass_kernel_spmd

F32 = mybir.dt.float32
BF16 = mybir.dt.bfloat16
I32 = mybir.dt.int32
ALU = mybir.AluOpType
AF = mybir.ActivationFunctionType
AX = mybir.AxisListType

NCORES = 8
S = 2048
D = 1024
E = 32
BLK = 512
ALPHA = float(4 ** 0.25)
EPS = 1e-5
WINS = (2, 4, 8, 16)

ENGS = ("pe", "act", "dve", "pool", "sp")
NDMASEM = 6
SEM_WRAP = 30000


class Buf:
    __slots__ = ("w", "r", "rd")

    def __init__(self):
        self.w = None
        self.r = {}
        self.rd = []


def bufs(n):
    return [Buf() for _ in range(n)]


class Op:
    __slots__ = ("eng", "fn", "deps", "dma", "need", "sem", "val", "phase")

    def __init__(self, eng, fn, dma, phase):
        self.eng = eng
        self.fn = fn
        self.deps = []
        self.dma = dma
        self.need = False
        self.sem = None
        self.val = 0
        self.phase = phase


class Sched:
    def __init__(self, nc):
        self.nc = nc
        self.ops = {e: [] for e in ENGS}
        self.prog = {}
        self.dma_sems = {}
        self.dma_n = {e: 0 for e in ENGS}
        self.nsem = 0
        self.phase = 0
        self.pending_dma = {e: [] for e in ENGS}
        self.deferred = {e: [] for e in ENGS}
        self.nops = 0

    def _newsem(self, tag):
        self.nsem += 1
        return self.nc.alloc_semaphore(f"{tag}_{self.nsem}")

    def collect_deferred(self):
        for e in ENGS:
            self.pending_dma[e].extend(self.deferred[e])
            self.deferred[e] = []

    def op(self, eng, fn, reads=(), writes=(), dma=False, defer=False):
        o = Op(eng, fn, dma, self.phase)
        deps = []
        for b in reads:
            if b.w is not None:
                deps.append(b.w)
        for b in writes:
            if b.w is not None:
                deps.append(b.w)
            deps.extend(b.r.values())
            deps.extend(b.rd)
        seen = set()
        for d in deps:
            if d is o or d.phase != self.phase or id(d) in seen:
                continue
            seen.add(id(d))
            if eng == "pe" and d.eng == "pe" and not d.dma and not dma:
                continue
            o.deps.append(d)
        for b in writes:
            b.w = o
            b.r = {}
            b.rd = []
        for b in reads:
            if dma:
                b.rd.append(o)
            else:
                b.r[eng] = o
        self.ops[eng].append(o)
        if dma:
            (self.deferred if defer else self.pending_dma)[eng].append(o)
        self.nops += 1
        return o

    def emit(self):
        nc = self.nc
        ops = self.ops
        for e in ENGS:
            for o in ops[e]:
                for d in o.deps:
                    d.need = True
                if o.dma:
                    o.need = True
        for e in ENGS:
            for o in ops[e]:
                if o.dma:
                    if e not in self.dma_sems:
                        self.dma_sems[e] = [[self._newsem("d" + e), 0] for _ in range(NDMASEM)]
                    slot = self.dma_sems[e][self.dma_n[e] % NDMASEM]
                    self.dma_n[e] += 1
                    slot[1] += 16
                    o.sem, o.val = slot[0], slot[1]
                elif o.need:
                    p = self.prog.get(e)
                    if p is None or p[1] >= SEM_WRAP:
                        p = [self._newsem("p" + e), 0]
                        self.prog[e] = p
                    p[1] += 1
                    o.sem, o.val = p[0], p[1]
        engmap = {"pe": "tensor", "act": "scalar", "dve": "vector", "pool": "gpsimd", "sp": "sync"}
        with nc.Block() as block:
            for e in ENGS:
                lst = ops[e]
                pend = self.pending_dma[e]

                def body(eng, lst=lst, pend=pend):
                    seen = {}

                    def wait(sem, val):
                        k = sem.num
                        if seen.get(k, 0) >= val:
                            return
                        seen[k] = val
                        eng.wait_ge(sem, val)

                    for o in lst:
                        for d in o.deps:
                            wait(d.sem, d.val)
                        if o.dma and o.val > 16:
                            wait(o.sem, o.val - 16)
                        ins = o.fn(eng)
                        if o.need:
                            ins.then_inc(o.sem, 16 if o.dma else 1)
                    for o in pend:
                        wait(o.sem, o.val)

                if lst or pend:
                    getattr(block, engmap[e])(body)
        self.ops = {e: [] for e in ENGS}
        self.pending_dma = {e: [] for e in ENGS}
        self.phase += 1


C_IDENT = 0
C_LSTRICT = 128
C_ONES = 256
C_BAND = 384
C_CAUSAL = C_BAND + 12 * 128
C_EC = C_CAUSAL + 128
C_SEL = C_EC + 32
NCONST = C_SEL + 12 * 65


def make_consts(cap):
    c = np.zeros((128, NCONST), np.float32)
    i = np.arange(128)
    c[:, C_IDENT:C_IDENT + 128] = np.eye(128, dtype=np.float32)
    c[:, C_LSTRICT:C_LSTRICT + 128] = (i[:, None] < i[None, :]).astype(np.float32)
    c[:, C_ONES:C_ONES + 128] = 1.0
    tp = i[:, None]
    t = i[None, :]
    for g, w in enumerate(WINS):
        diag = ((tp <= t) & (tp > t - w)).astype(np.float32) / w - (tp == t)
        off = ((tp - 128) > (t - w)).astype(np.float32) / w
        cnt = np.minimum(t + 1, w).astype(np.float32)
        first = ((tp <= t) & (tp > t - w)).astype(np.float32) / cnt - (tp == t)
        base = C_BAND + g * 3 * 128
        c[:, base:base + 128] = diag
        c[:, base + 128:base + 256] = off
        c[:, base + 256:base + 384] = first
    c[:, C_CAUSAL:C_CAUSAL + 128] = (tp <= t).astype(np.float32)
    c[:, C_EC:C_EC + 32] = (np.arange(32) * cap)[None, :].astype(np.float32)
    for h in range(12):
        c[h, C_SEL + h * 65 + 64] = 1.0
    return c


def build(NB=4, CAP=1536, debug=False, stop_after=None):
    T = NB * S
    NT = T // 128
    NSLOT = E * CAP
    NBLK = CAP // BLK
    nc = bass.Bass("TRN2", target_bir_lowering=False)
    sc = Sched(nc)

    def dram_in(name, shape, dt=F32):
        return nc.dram_tensor(name, shape, dt, kind="ExternalInput").ap()

    x_d = dram_in("x", [NB, S, D])
    mem_d = dram_in("mem", [NB, 256, D])
    ln1g_d = dram_in("ln1_g", [2, D]); ln1b_d = dram_in("ln1_b", [2, D])
    ln2g_d = dram_in("ln2_g", [2, D]); ln2b_d = dram_in("ln2_b", [2, D])
    awin_d = dram_in("a_w_in", [D, D])
    poolw_d = dram_in("pool_w", [4, 192, 192])
    pscale_d = dram_in("pool_scale", [768, 1])
    amkv_d = dram_in("a_mem_kv", [D, 512])
    awout_d = dram_in("a_w_out", [D, D])
    kvw_d = dram_in("kv_w", [D, 1548])
    fgb_d = dram_in("fgate_b", [12, 1])
    bwin_d = dram_in("b_w_in", [D, D])
    bmkv_d = dram_in("b_mem_kv", [D, 512])
    bwout_d = dram_in("b_w_out", [D, D])
    rw_d = dram_in("router_w", [2, D, E])
    rb_d = dram_in("router_b", [2, E])
    w1_d = dram_in("moe_w1", [2, E, D, 2048])
    b1_d = dram_in("moe_b1", [2, E, 2048])
    w2_d = dram_in("moe_w2", [2, E, D, D])
    b2_d = dram_in("moe_b2", [2, E, D])
    cst_d = dram_in("consts", [128, NCONST])
    out_d = nc.dram_tensor("out", [NB, S, D], F32, kind="ExternalOutput").ap()
    skind = "ExternalOutput" if debug else "Internal"
    H1_d = nc.dram_tensor("H1", [T, D], F32, kind=skind).ap()
    H2_d = nc.dram_tensor("H2", [T, D], F32, kind=skind).ap()
    XS_d = nc.dram_tensor("XS", [NSLOT, D], BF16, kind="Internal").ap()
    YS_d = nc.dram_tensor("YS", [NSLOT, D], F32, kind=skind).ap()
    if debug:
        DBGI_d = nc.dram_tensor("DBGI", [128, NT * 4], I32, kind="ExternalOutput").ap()
        DBGG_d = nc.dram_tensor("DBGG", [128, NT * 4], F32, kind="ExternalOutput").ap()
    B_H1 = Buf(); B_H2 = Buf(); B_XS = Buf(); B_YS = Buf(); B_OUT = Buf()

    uid = [0]

    def sb(es, name, shape, dt):
        uid[0] += 1
        return es.enter_context(nc.sbuf_tensor(f"{name}_{uid[0]}", shape, dt))

    def ps(es, name, shape, dt=F32):
        uid[0] += 1
        return es.enter_context(nc.psum_tensor(f"{name}_{uid[0]}", shape, dt))

    op = sc.op
    bcreg = nc.gpsimd.alloc_register("bcreg")

    def set_bc():
        op("pool", lambda e: e.reg_mov(bcreg, NSLOT - 1))

    with ExitStack() as gs:
        cst = sb(gs, "cst", [128, NCONST], F32)
        cstb = sb(gs, "cstb", [128, C_EC], BF16)
        idx_all = sb(gs, "idx_all", [128, NT, 4], I32)
        gate_all = sb(gs, "gate_all", [128, NT, 4], F32)
        base_cnt = sb(gs, "base_cnt", [128, E], F32)
        b1T = sb(gs, "b1T", [128, 16, E], F32)
        epsc = sb(gs, "epsc", [128, 1], F32)
        B_CST = Buf(); B_IDX = bufs(NT); B_GATE = bufs(NT); B_BASE = Buf(); B_B1T = Buf()

        identf = cst[:, C_IDENT:C_IDENT + 128]
        identb = cstb[:, C_IDENT:C_IDENT + 128]
        onesf = cst[:, C_ONES:C_ONES + 128]
        onesb = cstb[:, C_ONES:C_ONES + 128]
        lstrict = cst[:, C_LSTRICT:C_LSTRICT + 128]
        causalb = cstb[:, C_CAUSAL:C_CAUSAL + 128]

        def band(g, k):
            o = C_BAND + (g * 3 + k) * 128
            return cstb[:, o:o + 128]

        zt = sb(gs, "zt", [128, 4096], BF16)
        BZ = Buf()
        with ExitStack() as es:
            op("sp", lambda e: e.dma_start(out=cst[:], in_=cst_d[:, :]), writes=[B_CST], dma=True)
            op("dve", lambda e: e.tensor_copy(cstb[:], cst[:, 0:C_EC]), reads=[B_CST], writes=[B_CST])
            op("dve", lambda e: e.memset(base_cnt[:], 0.0), writes=[B_BASE])
            op("dve", lambda e: e.memset(epsc[:], EPS), writes=[B_CST])
            op("pool", lambda e: e.memset(zt[:], 0.0), writes=[BZ])
            sc.emit()

        def load_w_bf16(dst, src_ap, wbuf):
            op("pool", lambda e: e.dma_start(out=dst, in_=src_ap), writes=[wbuf], dma=True)

        def hT_res(es, tag):
            xt = [sb(es, f"xt{tag}{i}", [128, D], F32) for i in range(2)]
            ptr = [ps(es, f"ptr{tag}{i}", [128, 8, 128], F32) for i in range(2)]
            return (xt, bufs(2), ptr, bufs(2))

        def make_hT(res, src_rows, hT, HB, nt):
            xt, XB, ptr, PB = res
            for t in range(nt):
                i = t % 2
                op("sp", lambda e, t=t, i=i: e.dma_start(out=xt[i][:], in_=src_rows(t)), writes=[XB[i]], dma=True)
                for kc in range(8):
                    op("pe", lambda e, i=i, kc=kc: e.transpose(ptr[i][:, kc, :], xt[i][:, kc * 128:(kc + 1) * 128], identf),
                       reads=[XB[i], B_CST], writes=[PB[i]])
                eng = "act" if t % 2 == 0 else "dve"
                if eng == "act":
                    op("act", lambda e, t=t, i=i: e.copy(hT[:, :, t * 128:(t + 1) * 128], ptr[i][:, :, :]),
                       reads=[PB[i]], writes=[HB[t]])
                else:
                    op("dve", lambda e, t=t, i=i: e.tensor_copy(hT[:, :, t * 128:(t + 1) * 128], ptr[i][:, :, :]),
                       reads=[PB[i]], writes=[HB[t]])

        def mem_kv(es, b, wkv_d, res, mkT, mv, B_MK, B_MV, tag):
            wkv = sb(es, f"wkv{tag}", [128, 8, 512], BF16); BW = Buf()
            load_w_bf16(wkv[:], wkv_d.rearrange("(kc p) f -> p kc f", p=128), BW)
            memT = sb(es, f"memT{tag}", [128, 8, 256], BF16); MB = bufs(2)
            make_hT(res, lambda t: mem_d[b, t * 128:(t + 1) * 128, :], memT, MB, 2)
            pm = ps(es, f"pm{tag}", [128, 512], F32); PMB = Buf()
            op("pool", lambda e: e.memset(mv[:], 1.0), writes=[B_MV])
            for m in range(2):
                for kc in range(8):
                    op("pe", lambda e, m=m, kc=kc: e.matmul(pm[:, 0:256], wkv[:, kc, m * 128:(m + 1) * 128], memT[:, kc, :],
                                                            start=(kc == 0), stop=(kc == 7)),
                       reads=[BW, MB[0], MB[1]], writes=[PMB])
                op("dve", lambda e, m=m: e.tensor_copy(mkT[:, m, :], pm[:, 0:256]), reads=[PMB], writes=[B_MK])
            for mc in range(2):
                for kc in range(8):
                    op("pe", lambda e, mc=mc, kc=kc: e.matmul(pm[:, 0:256], memT[:, kc, mc * 128:(mc + 1) * 128], wkv[:, kc, 256:512],
                                                              start=(kc == 0), stop=(kc == 7)),
                       reads=[BW, MB[mc]], writes=[PMB])
                for h in range(4):
                    c0 = 0 if h % 2 == 0 else 64
                    op("dve", lambda e, mc=mc, h=h, c0=c0: e.tensor_copy(mv[:, mc, h, c0:c0 + 64], pm[:, h * 64:(h + 1) * 64]),
                       reads=[PMB], writes=[B_MV])

        def mem_attn(es, qmT, B_QM, mkT, mv, B_MK, B_MV, mixT, mix_chunk0, B_MIX, tag):
            psc = [ps(es, f"msc{tag}{i}", [128, 512], F32) for i in range(2)]; PSC = bufs(2)
            po = [ps(es, f"mo{tag}{i}", [128, 512], F32) for i in range(2)]; PO = bufs(2)
            pt = [sb(es, f"mpt{tag}{i}", [128, 512], BF16) for i in range(3)]; PT = bufs(3)
            rc = [sb(es, f"mrc{tag}{i}", [128, 512], F32) for i in range(2)]; RC = bufs(2)
            its = []
            no = 0
            for h in range(4):
                for sbk in range(S // 512):
                    io = no % 2
                    no += 1
                    for mc in range(2):
                        its.append((h, sbk, io, mc))

            def emit_qk(n):
                h, sbk, io, mc = its[n]
                m = h // 2
                r0 = (h % 2) * 64
                cs = slice(sbk * 512, (sbk + 1) * 512)
                i2 = n % 2
                op("pe", lambda e: e.matmul(
                    psc[i2][:], mkT[r0:r0 + 64, m, mc * 128:(mc + 1) * 128], qmT[r0:r0 + 64, m, cs],
                    start=True, stop=True), reads=[B_MK, B_QM], writes=[PSC[i2]])

            def emit_pv(n):
                h, sbk, io, mc = its[n]
                m = h // 2
                r0 = (h % 2) * 64
                d0 = 64 - r0
                cs = slice(sbk * 512, (sbk + 1) * 512)
                i2 = n % 2
                i3 = n % 3
                op("act", lambda e: e.activation(pt[i3][:], psc[i2][:], AF.Exp, scale=0.125),
                   reads=[PSC[i2]], writes=[PT[i3]])
                op("pe", lambda e: e.matmul(
                    po[io][:], mv[:, mc, h, :], pt[i3][:], start=(mc == 0), stop=(mc == 1)),
                    reads=[B_MV, PT[i3]], writes=[PO[io]])
                if mc == 1:
                    op("dve", lambda e: e.reciprocal(rc[io][r0:r0 + 64, :], po[io][d0:d0 + 64, :]),
                       reads=[PO[io]], writes=[RC[io]])
                    op("dve", lambda e: e.tensor_tensor(
                        mixT[r0:r0 + 64, mix_chunk0 + m, cs], po[io][r0:r0 + 64, :], rc[io][r0:r0 + 64, :], ALU.mult),
                        reads=[PO[io], RC[io]], writes=[B_MIX])

            for n in range(len(its) + 1):
                if n < len(its):
                    emit_qk(n)
                if n - 1 >= 0:
                    emit_pv(n - 1)

        def ln_bc(es, g_d, b_d, l, tag):
            g_bc = sb(es, f"g_bc{tag}", [128, D], F32)
            b_bc = sb(es, f"b_bc{tag}", [128, D], F32)
            BG = Buf()
            op("sp", lambda e: e.dma_start(out=g_bc[:], in_=g_d[l:l + 1, :].to_broadcast([128, D])), writes=[BG], dma=True)
            op("sp", lambda e: e.dma_start(out=b_bc[:], in_=b_d[l:l + 1, :].to_broadcast([128, D])), writes=[BG], dma=True)
            return g_bc, b_bc, BG

        def layer_norm(z, ZB, out, OB, g_bc, b_bc, BG, st, STB, g_eng="dve"):
            stats, mv, rstd, nmr = st
            for c in range(2):
                op("dve", lambda e, c=c: e.bn_stats(stats[:, c * 6:(c + 1) * 6], z[:, c * 512:(c + 1) * 512]), reads=[ZB], writes=[STB])
            op("dve", lambda e: e.bn_aggr(mv[:], stats[:]), reads=[STB], writes=[STB])
            op("act", lambda e: e.activation(rstd[:], mv[:, 1:2], AF.Ln, bias=epsc[:, 0:1], scale=1.0), reads=[STB, B_CST], writes=[STB])
            op("act", lambda e: e.activation(rstd[:], rstd[:], AF.Exp, scale=-0.5), reads=[STB], writes=[STB])
            op("dve", lambda e: e.scalar_tensor_tensor(out=nmr[:], in0=mv[:, 0:1], scalar=-1.0, in1=rstd[:], op0=ALU.mult, op1=ALU.mult),
               reads=[STB], writes=[STB])
            op("act", lambda e: e.activation(z[:], z[:], AF.Identity, bias=nmr[:, 0:1], scale=rstd[:, 0:1]), reads=[ZB, STB], writes=[ZB])
            if g_eng == "pool":
                op("pool", lambda e: e.tensor_tensor(out=z[:], in0=z[:], in1=g_bc[:], op=ALU.mult), reads=[ZB, BG], writes=[ZB])
            else:
                op("dve", lambda e: e.tensor_tensor(z[:], z[:], g_bc[:], ALU.mult), reads=[ZB, BG], writes=[ZB])
            op("dve", lambda e: e.tensor_tensor(out[:], z[:], b_bc[:], ALU.add), reads=[ZB, BG], writes=[OB])

        def outproj_ln_router(es, l, b, mixT, B_MIX, kchunks, wout_d, h_rows):
            nk = len(kchunks)
            set_bc()
            wout = sb(es, "wout", [128, nk, D], BF16); BW = Buf()
            wout_d(wout, BW)
            g_bc, b_bc, BG = ln_bc(es, ln1g_d, ln1b_d, l, "1")
            rw = sb(es, "rw", [128, 8, E], F32); BR = Buf()
            op("sp", lambda e: e.dma_start(out=rw[:], in_=rw_d[l].rearrange("(kc p) e -> p kc e", p=128)), writes=[BR], dma=True)
            rb_bc = sb(es, "rb_bc", [128, E], F32)
            op("sp", lambda e: e.dma_start(out=rb_bc[:], in_=rb_d[l:l + 1, :].to_broadcast([128, E])), writes=[BR], dma=True)
            NBUF = 3
            ht = [sb(es, f"ht{i}", [128, D], F32) for i in range(NBUF)]; HTB = bufs(NBUF)
            z = [sb(es, f"z{i}", [128, D], F32) for i in range(NBUF)]; ZB = bufs(NBUF)
            h1 = [sb(es, f"h1{i}", [128, D], F32) for i in range(NBUF)]; H1B = bufs(NBUF)
            h1b = [sb(es, f"h1b{i}", [128, D], BF16) for i in range(NBUF)]; H1BB = bufs(NBUF)
            h1T = [sb(es, f"h1T{i}", [128, 8, 128], F32) for i in range(2)]; H1TB = bufs(2)
            st = [(sb(es, f"st{i}", [128, 12], F32), sb(es, f"mv{i}", [128, 2], F32),
                   sb(es, f"rstd{i}", [128, 1], F32), sb(es, f"nmr{i}", [128, 1], F32)) for i in range(NBUF)]
            STB = bufs(NBUF)
            po = [ps(es, f"po{i}", [128, D], F32) for i in range(2)]; POB = bufs(2)
            ptr = ps(es, "ptrr", [128, 8, 128], F32); PTRB = Buf()
            pr = ps(es, "prt", [128, 512], F32)
            PLGB = bufs(2); PCMB = Buf()
            plg = [pr[:, 0:E], pr[:, 64:64 + E]]
            pcm = pr[:, 128:128 + 2 * E]
            NR = 2
            rt = []
            for i in range(NR):
                rt.append(dict(
                    lg=sb(es, f"lg{i}", [128, E], F32), top8=sb(es, f"top8{i}", [128, 8], F32),
                    ex=sb(es, f"ex{i}", [128, 4], F32), ssum=sb(es, f"ssum{i}", [128, 1], F32), nv0=sb(es, f"nv0{i}", [128, 1], F32),
                    Mm=sb(es, f"Mm{i}", [128, E], F32), sbase=sb(es, f"sbase{i}", [128, E], F32),
                    oh=sb(es, f"oh{i}", [128, 4, E], F32), slotf=sb(es, f"slotf{i}", [128, 4], F32)))
            RBS = bufs(NR)
            NTL = S // 128

            def S1(tl):
                i = tl % NBUF
                ip = tl % 2
                t0 = tl * 128
                for hf in range(2):
                    for k, (ci, rows, ro) in enumerate(kchunks):
                        op("pe", lambda e, hf=hf, k=k, ci=ci, rows=rows: e.matmul(
                            po[ip][:, hf * 512:(hf + 1) * 512], mixT[0:rows, ci, t0:t0 + 128], wout[0:rows, k, hf * 512:(hf + 1) * 512],
                            start=(k == 0), stop=(k == nk - 1)), reads=[B_MIX, BW], writes=[POB[ip]])
                op("sp", lambda e: e.dma_start(out=ht[i][:], in_=h_rows(tl)), writes=[HTB[i]], dma=True)

            def S1b(tl):
                i = tl % NBUF
                ip = tl % 2
                for hf in range(2):
                    op("dve", lambda e, hf=hf: e.scalar_tensor_tensor(
                        out=z[i][:, hf * 512:(hf + 1) * 512], in0=ht[i][:, hf * 512:(hf + 1) * 512], scalar=ALPHA,
                        in1=po[ip][:, hf * 512:(hf + 1) * 512], op0=ALU.mult, op1=ALU.add),
                        reads=[HTB[i], POB[ip]], writes=[ZB[i]])
                stats, mv, rstd, nmr = st[i]
                for c in range(2):
                    op("dve", lambda e, c=c: e.bn_stats(stats[:, c * 6:(c + 1) * 6], z[i][:, c * 512:(c + 1) * 512]), reads=[ZB[i]], writes=[STB[i]])
                op("dve", lambda e: e.bn_aggr(mv[:], stats[:]), reads=[STB[i]], writes=[STB[i]])
                op("act", lambda e: e.activation(rstd[:], mv[:, 1:2], AF.Ln, bias=epsc[:, 0:1], scale=1.0), reads=[STB[i], B_CST], writes=[STB[i]])
                op("act", lambda e: e.activation(rstd[:], rstd[:], AF.Exp, scale=-0.5), reads=[STB[i]], writes=[STB[i]])
                op("dve", lambda e: e.scalar_tensor_tensor(out=nmr[:], in0=mv[:, 0:1], scalar=-1.0, in1=rstd[:], op0=ALU.mult, op1=ALU.mult),
                   reads=[STB[i]], writes=[STB[i]])

            def S2(tl):
                i = tl % NBUF
                i2 = tl % 2
                tt = b * NTL + tl
                stats, mv, rstd, nmr = st[i]
                op("act", lambda e: e.activation(z[i][:], z[i][:], AF.Identity, bias=nmr[:, 0:1], scale=rstd[:, 0:1]), reads=[ZB[i], STB[i]], writes=[ZB[i]])
                op("dve", lambda e: e.tensor_tensor(z[i][:], z[i][:], g_bc[:], ALU.mult), reads=[ZB[i], BG], writes=[ZB[i]])
                op("dve", lambda e: e.tensor_tensor(h1[i][:], z[i][:], b_bc[:], ALU.add), reads=[ZB[i], BG], writes=[H1B[i]])
                op("sp", lambda e: e.dma_start(out=H1_d[tt * 128:(tt + 1) * 128, :], in_=h1[i][:]),
                   reads=[H1B[i]], writes=[B_H1], dma=True)
                op("act", lambda e: e.copy(h1b[i][:], h1[i][:]), reads=[H1B[i]], writes=[H1BB[i]])
                for kc in range(8):
                    op("pe", lambda e, kc=kc: e.transpose(ptr[:, kc, :], h1[i][:, kc * 128:(kc + 1) * 128], identf),
                       reads=[H1B[i], B_CST], writes=[PTRB])
                op("act", lambda e: e.copy(h1T[i2][:, :, :], ptr[:, :, :]), reads=[PTRB], writes=[H1TB[i2]])
                for kc in range(8):
                    op("pe", lambda e, kc=kc: e.matmul(plg[i2], h1T[i2][:, kc, :], rw[:, kc, :], start=(kc == 0), stop=(kc == 7)),
                       reads=[H1TB[i2], BR], writes=[PLGB[i2]])

            def S3(tl):
                i = tl % NBUF
                i2 = tl % 2
                tt = b * NTL + tl
                r = rt[tl % NR]; RB = RBS[tl % NR]
                lg, top8, ex, ssum, nv0, Mm, sbase, oh, slotf = (r[k] for k in ("lg", "top8", "ex", "ssum", "nv0", "Mm", "sbase", "oh", "slotf"))
                op("dve", lambda e: e.tensor_tensor(lg[:], plg[i2], rb_bc[:], ALU.add), reads=[PLGB[i2], BR], writes=[RB])
                op("dve", lambda e: e.max(top8[:], lg[:]), reads=[RB], writes=[RB])
                op("dve", lambda e: e.tensor_scalar(Mm[:], lg[:], top8[:, 3:4], None, ALU.is_ge), reads=[RB], writes=[RB])
                op("pe", lambda e: e.matmul(pcm[:, 0:E], lstrict, Mm[:], start=True, stop=True), reads=[RB, B_CST], writes=[PCMB])
                op("pe", lambda e: e.matmul(pcm[:, E:2 * E], onesf, Mm[:], start=True, stop=True), reads=[RB, B_CST], writes=[PCMB])
                op("dve", lambda e: e.tensor_scalar(nv0[:], top8[:, 0:1], -1.0, None, ALU.mult), reads=[RB], writes=[RB])
                op("act", lambda e: e.activation(ex[:], top8[:, 0:4], AF.Exp, bias=nv0[:, 0:1], scale=1.0), reads=[RB], writes=[RB])
                op("dve", lambda e: e.reduce_sum(ssum[:], ex[:], axis=AX.X), reads=[RB], writes=[RB])
                op("dve", lambda e: e.reciprocal(ssum[:], ssum[:]), reads=[RB], writes=[RB])
                op("dve", lambda e: e.tensor_scalar(gate_all[:, tt, :], ex[:], ssum[:, 0:1], None, ALU.mult),
                   reads=[RB], writes=[B_GATE[tt]])
                op("dve", lambda e: e.tensor_tensor(sbase[:], pcm[:, 0:E], base_cnt[:], ALU.add), reads=[PCMB, B_BASE], writes=[RB])
                op("dve", lambda e: e.tensor_tensor(base_cnt[:], pcm[:, E:2 * E], base_cnt[:], ALU.add), reads=[PCMB, B_BASE], writes=[B_BASE])
                op("dve", lambda e: e.tensor_scalar(sbase[:], sbase[:], float(CAP - 1), None, ALU.min), reads=[RB], writes=[RB])
                op("dve", lambda e: e.tensor_tensor(sbase[:], sbase[:], cst[:, C_EC:C_EC + E], ALU.add), reads=[RB, B_CST], writes=[RB])
                lg_b = lg[:].unsqueeze(1).to_broadcast([128, 4, E])
                tv_b = top8[:, 0:4].unsqueeze(2).to_broadcast([128, 4, E])
                sb_b = sbase[:].unsqueeze(1).to_broadcast([128, 4, E])
                op("dve", lambda e: e.tensor_tensor(oh[:, :, :], lg_b, tv_b, ALU.is_equal), reads=[RB], writes=[RB])
                op("dve", lambda e: e.tensor_tensor(oh[:, :, :], oh[:, :, :], sb_b, ALU.mult), reads=[RB], writes=[RB])
                op("dve", lambda e: e.tensor_reduce(out=slotf[:], in_=oh[:, :, :], axis=AX.X, op=ALU.add), reads=[RB], writes=[RB])
                op("dve", lambda e: e.tensor_copy(idx_all[:, tt, :], slotf[:]), reads=[RB], writes=[B_IDX[tt]])
                for k in range(4):
                    op("pool", lambda e, k=k: e.indirect_dma_start(
                        out=XS_d[:, :], out_offset=bass.IndirectOffsetOnAxis(ap=idx_all[:, tt, k:k + 1], axis=0),
                        in_=h1b[i][:, :], in_offset=None, bounds_check=bcreg, oob_is_err=False),
                        reads=[H1BB[i], B_IDX[tt]], writes=[B_XS], dma=True)

            for n in range(NTL + 2):
                if n < NTL:
                    S1(n)
                if 0 <= n - 2 < NTL:
                    S3(n - 2)
                if 0 <= n - 1 < NTL:
                    S2(n - 1)
                if n < NTL:
                    S1b(n)

        def mixer_A(b):
            with ExitStack() as e0:
                mixT = sb(e0, "mixT", [128, 10, S], BF16); B_MIX = Buf()
                with ExitStack() as e1:
                    u = sb(e1, "u", [128, 16, 768], BF16); UB = bufs(16)
                    qmT = sb(e1, "qmT", [128, 2, S], BF16); B_QM = Buf()
                    mkT = sb(e1, "mkT", [128, 2, 256], BF16); B_MK = Buf()
                    mv = sb(e1, "mv", [128, 2, 4, 128], BF16); B_MV = Buf()
                    with ExitStack() as es:
                        if b == 0:
                            zero_fill_bg()
                        hT = sb(es, "hT", [128, 8, S], BF16); HB = bufs(16)
                        win = sb(es, "win", [128, 8, D], BF16); BW = Buf()
                        load_w_bf16(win[:], awin_d.rearrange("(kc p) f -> p kc f", p=128), BW)
                        res = hT_res(es, "a")
                        make_hT(res, lambda t: x_d[b, t * 128:(t + 1) * 128, :], hT, HB, 16)
                        mem_kv(es, b, amkv_d, res, mkT, mv, B_MK, B_MV, "a")
                        pu = ps(es, "pu", [128, 1024], F32); PUB = Buf()
                        pq = ps(es, "pq", [128, 512], F32); PQB = Buf()
                        for t in range(16):
                            for (c0, c1) in ((0, 512), (512, 768)):
                                for kc in range(8):
                                    op("pe", lambda e, t=t, c0=c0, c1=c1, kc=kc: e.matmul(
                                        pu[:, c0:c1], hT[:, kc, t * 128:(t + 1) * 128], win[:, kc, c0:c1], start=(kc == 0), stop=(kc == 7)),
                                        reads=[HB[t], BW], writes=[PUB])
                            op("act", lambda e, t=t: e.copy(u[:, t, :], pu[:, 0:768]), reads=[PUB], writes=[UB[t]])
                        for m in range(2):
                            for sbk in range(4):
                                for kc in range(8):
                                    op("pe", lambda e, m=m, sbk=sbk, kc=kc: e.matmul(
                                        pq[:], win[:, kc, 768 + m * 128:768 + (m + 1) * 128], hT[:, kc, sbk * 512:(sbk + 1) * 512],
                                        start=(kc == 0), stop=(kc == 7)), reads=[HB[4 * sbk + j] for j in range(4)] + [BW], writes=[PQB])
                                op("dve", lambda e, m=m, sbk=sbk: e.tensor_copy(qmT[:, m, sbk * 512:(sbk + 1) * 512], pq[:]),
                                   reads=[PQB], writes=[B_QM])
                        sc.emit()
                    with ExitStack() as es:
                        dT = sb(es, "dT", [128, 8, S], BF16); DB = Buf()
                        pw = sb(es, "pw", [128, 4, 2, 192], BF16); BPW = Buf()
                        for g in range(4):
                            load_w_bf16(pw[:, g, 0, :], poolw_d[g, 0:128, :], BPW)
                            load_w_bf16(pw[0:64, g, 1, :], poolw_d[g, 128:192, :], BPW)
                        psc_t = sb(es, "psc_t", [128, 8], F32); BPS = Buf()
                        for g in range(4):
                            op("sp", lambda e, g=g: e.dma_start(out=psc_t[:, 2 * g:2 * g + 1], in_=pscale_d[192 * g:192 * g + 128, :]),
                               writes=[BPS], dma=True)
                            op("sp", lambda e, g=g: e.dma_start(out=psc_t[0:64, 2 * g + 1:2 * g + 2], in_=pscale_d[192 * g + 128:192 * g + 192, :]),
                               writes=[BPS], dma=True)
                        pd = [ps(es, f"pd{i}", [128, 512], F32) for i in range(2)]; PDB = bufs(2)
                        n = 0
                        for g in range(4):
                            for ch, (c0, rows) in enumerate(((0, 128), (128, 64))):
                                cc = 192 * g + c0
                                for q4 in range(4):
                                    i = n % 2
                                    n += 1
                                    for j in range(4):
                                        t = q4 * 4 + j
                                        if t == 0:
                                            op("pe", lambda e, i=i, j=j, t=t, cc=cc, rows=rows, g=g: e.matmul(
                                                pd[i][0:rows, j * 128:(j + 1) * 128], u[:, t, cc:cc + rows], band(g, 2), start=True, stop=True),
                                                reads=[UB[t], B_CST], writes=[PDB[i]])
                                        else:
                                            op("pe", lambda e, i=i, j=j, t=t, cc=cc, rows=rows, g=g: e.matmul(
                                                pd[i][0:rows, j * 128:(j + 1) * 128], u[:, t, cc:cc + rows], band(g, 0), start=True, stop=False),
                                                reads=[UB[t], B_CST], writes=[PDB[i]])
                                            op("pe", lambda e, i=i, j=j, t=t, cc=cc, rows=rows, g=g: e.matmul(
                                                pd[i][0:rows, j * 128:(j + 1) * 128], u[:, t - 1, cc:cc + rows], band(g, 1), start=False, stop=True),
                                                reads=[UB[t - 1], B_CST], writes=[PDB[i]])
                                    eng = "act" if n % 2 == 0 else "dve"
                                    if eng == "act":
                                        op("act", lambda e, i=i, rows=rows, g=g, ch=ch, q4=q4: e.copy(
                                            dT[0:rows, 2 * g + ch, q4 * 512:(q4 + 1) * 512], pd[i][0:rows, :]), reads=[PDB[i]], writes=[DB])
                                    else:
                                        op("dve", lambda e, i=i, rows=rows, g=g, ch=ch, q4=q4: e.tensor_copy(
                                            dT[0:rows, 2 * g + ch, q4 * 512:(q4 + 1) * 512], pd[i][0:rows, :]), reads=[PDB[i]], writes=[DB])
                        py = [ps(es, f"py{i}", [128, 512], F32) for i in range(2)]; PYB = bufs(2)
                        n = 0
                        for g in range(4):
                            for och, (o0, orows) in enumerate(((0, 128), (128, 64))):
                                for q4 in range(4):
                                    i = n % 2
                                    n += 1
                                    for ich, irows in enumerate((128, 64)):
                                        op("pe", lambda e, i=i, g=g, o0=o0, orows=orows, ich=ich, irows=irows, q4=q4: e.matmul(
                                            py[i][0:orows, :], pw[0:irows, g, ich, o0:o0 + orows], dT[0:irows, 2 * g + ich, q4 * 512:(q4 + 1) * 512],
                                            start=(ich == 0), stop=(ich == 1)), reads=[BPW, DB], writes=[PYB[i]])
                                    op("act", lambda e, i=i, g=g, och=och, orows=orows, q4=q4: e.activation(
                                        mixT[0:orows, 2 * g + och, q4 * 512:(q4 + 1) * 512], py[i][0:orows, :], AF.Identity,
                                        scale=psc_t[0:orows, 2 * g + och:2 * g + och + 1]), reads=[PYB[i], BPS], writes=[B_MIX])
                        mem_attn(es, qmT, B_QM, mkT, mv, B_MK, B_MV, mixT, 8, B_MIX, "a")
                        if b == 0:
                            sc.collect_deferred()
                        sc.emit()
                with ExitStack() as es:
                    kch = [(2 * g, 128, 0) for g in range(4)] + [(2 * g + 1, 64, 0) for g in range(4)] + [(8, 128, 0), (9, 128, 0)]

                    def load_wout_a(wout, BW):
                        src = awout_d[0:768, :].rearrange("(g r) d -> r g d", r=192)
                        load_w_bf16(wout[:, 0:4, :], src[0:128, :, :], BW)
                        load_w_bf16(wout[0:64, 4:8, :], src[128:192, :, :], BW)
                        load_w_bf16(wout[:, 8:10, :], awout_d[768:1024, :].rearrange("(c p) d -> p c d", p=128), BW)
                    outproj_ln_router(es, 0, b, mixT, B_MIX, kch, load_wout_a, lambda tl: x_d[b, tl * 128:(tl + 1) * 128, :])
                    sc.emit()

        def mixer_B(b):
            hsrc = lambda t: H2_d[b * S + t * 128: b * S + (t + 1) * 128, :]
            with ExitStack() as e0:
                mixT = sb(e0, "mixTb", [128, 8, S], BF16); B_MIX = Buf()
                with ExitStack() as e1:
                    hT = sb(e1, "hTb", [128, 8, S], BF16); HB = bufs(16)
                    negc = sb(e1, "negc", [128, 16, 12], F32); B_NC = Buf()
                    c8T = sb(e1, "c8T", [12, S], BF16); B_C8 = Buf()
                    selb = sb(e1, "selb", [12, 12, 65], BF16); B_SEL = Buf()
                    with ExitStack() as es:
                        make_hT(hT_res(es, "b"), hsrc, hT, HB, 16)
                        wg = sb(es, "wg", [128, 8, 12], BF16); BW = Buf()
                        load_w_bf16(wg[:], kvw_d[:, 1536:1548].rearrange("(kc p) f -> p kc f", p=128), BW)
                        fgb = sb(es, "fgb", [12, 1], F32); BF_ = Buf()
                        op("sp", lambda e: e.dma_start(out=fgb[:], in_=fgb_d[:, :]), writes=[BF_], dma=True)
                        op("dve", lambda e: e.tensor_scalar(fgb[:], fgb[:], -1.0, None, ALU.mult), reads=[BF_], writes=[BF_])
                        op("dve", lambda e: e.tensor_copy(selb[:, :, :], cst[0:12, C_SEL:C_SEL + 12 * 65]), reads=[B_CST], writes=[B_SEL])
                        sp_t = sb(es, "sp_t", [12, S], F32); BSP = Buf()
                        cT = sb(es, "cT", [12, S], F32); BCT = Buf()
                        ones12 = sb(es, "ones12", [12, S], F32); BO = Buf()
                        op("pool", lambda e: e.memset(ones12[:], 1.0), writes=[BO])
                        pg = [ps(es, f"pg{i}", [128, 512], F32) for i in range(2)]; PGB = bufs(2)
                        for sbk in range(4):
                            i = sbk % 2
                            for kc in range(8):
                                op("pe", lambda e, i=i, sbk=sbk, kc=kc: e.matmul(
                                    pg[i][0:12, :], wg[:, kc, :], hT[:, kc, sbk * 512:(sbk + 1) * 512], start=(kc == 0), stop=(kc == 7)),
                                    reads=[BW] + [HB[4 * sbk + j] for j in range(4)], writes=[PGB[i]])
                            op("act", lambda e, i=i, sbk=sbk: e.activation(sp_t[:, sbk * 512:(sbk + 1) * 512], pg[i][0:12, :], AF.Exp,
                                                                           bias=fgb[:, 0:1], scale=-1.0), reads=[PGB[i], BF_], writes=[BSP])
                        op("act", lambda e: e.activation(sp_t[:], sp_t[:], AF.Ln, bias=1.0, scale=1.0), reads=[BSP], writes=[BSP])
                        op("dve", lambda e: e.tensor_tensor_scan(cT[:], ones12[:], sp_t[:], 0.0, ALU.mult, ALU.add), reads=[BSP, BO], writes=[BCT])
                        op("dve", lambda e: e.tensor_scalar(c8T[:], cT[:], -8.0, None, ALU.mult), reads=[BCT], writes=[B_C8])
                        pt = ps(es, "ptc", [128, 16, 12], F32); PTB = Buf()
                        for ch in range(16):
                            op("pe", lambda e, ch=ch: e.transpose(pt[:, ch, :], cT[:, ch * 128:(ch + 1) * 128], cst[0:12, C_IDENT:C_IDENT + 12]),
                               reads=[BCT, B_CST], writes=[PTB])
                        op("dve", lambda e: e.tensor_copy(negc[:, :, :], pt[:, :, :]), reads=[PTB], writes=[B_NC])
                        sc.emit()
                    for grp in range(3):
                        with ExitStack() as es:
                            h0 = grp * 4
                            kT = sb(es, "kT", [65, 4, S], BF16); B_KT = Buf()
                            qT = sb(es, "qT", [65, 4, S], BF16); B_QT = Buf()
                            va = sb(es, "va", [128, 16, 4, 128], BF16); B_VA = bufs(16)
                            wk = sb(es, "wk", [128, 8, 256], BF16); BWK = Buf()
                            wv = sb(es, "wv", [128, 8, 256], BF16); BWV = Buf()
                            wq = sb(es, "wq", [128, 8, 4, 65], BF16); BWQ = Buf()
                            load_w_bf16(wk[:], kvw_d[:, h0 * 64:(h0 + 4) * 64].rearrange("(kc p) f -> p kc f", p=128), BWK)
                            load_w_bf16(wv[:], kvw_d[:, 768 + h0 * 64:768 + (h0 + 4) * 64].rearrange("(kc p) f -> p kc f", p=128), BWV)
                            op("pool", lambda e: e.memset(wq[:], 0.0), writes=[BWQ])
                            for hh in range(4):
                                load_w_bf16(wq[:, :, hh, 0:64],
                                            bwin_d[:, (h0 + hh) * 64:(h0 + hh + 1) * 64].rearrange("(kc p) f -> p kc f", p=128), BWQ)
                            B_KT = bufs(4); B_QT = bufs(4)
                            op("pool", lambda e: e.memset(kT[64:65, :, :], 1.0), writes=B_KT)
                            op("pool", lambda e: e.memset(va[:], 1.0), writes=B_VA)
                            pp = [ps(es, f"pp{i}", [128, 512], F32) for i in range(2)]; PPB = bufs(2)
                            pcount = [0]

                            def kq_units(hh):
                                units = []
                                for sbk in range(4):
                                    cs = slice(sbk * 512, (sbk + 1) * 512)
                                    hbs = [HB[4 * sbk + j] for j in range(4)]

                                    def uk(cs=cs, hbs=hbs):
                                        i = pcount[0] % 2; pcount[0] += 1
                                        for kc in range(8):
                                            op("pe", lambda e, kc=kc: e.matmul(
                                                pp[i][0:64, :], wk[:, kc, hh * 64:(hh + 1) * 64], hT[:, kc, cs], start=(kc == 0), stop=(kc == 7)),
                                                reads=[BWK] + hbs, writes=[PPB[i]])
                                        op("dve", lambda e: e.tensor_copy(kT[0:64, hh, cs], pp[i][0:64, :]), reads=[PPB[i]], writes=[B_KT[hh]])

                                    def uq(cs=cs, hbs=hbs):
                                        i = pcount[0] % 2; pcount[0] += 1
                                        for kc in range(8):
                                            op("pe", lambda e, kc=kc: e.matmul(
                                                pp[i][0:65, :], wq[:, kc, hh, :], hT[:, kc, cs], start=(kc == 0), stop=False),
                                                reads=[BWQ] + hbs, writes=[PPB[i]])
                                        op("pe", lambda e: e.matmul(
                                            pp[i][0:65, :], selb[:, h0 + hh, :], c8T[:, cs], start=False, stop=True),
                                            reads=[B_SEL, B_C8], writes=[PPB[i]])
                                        op("dve", lambda e: e.tensor_copy(qT[0:65, hh, cs], pp[i][0:65, :]), reads=[PPB[i]], writes=[B_QT[hh]])
                                    units.append(uk)
                                    units.append(uq)
                                return units

                            for t in range(16):
                                i = pcount[0] % 2; pcount[0] += 1
                                for kc in range(8):
                                    op("pe", lambda e, i=i, t=t, kc=kc: e.matmul(
                                        pp[i][:, 0:256], hT[:, kc, t * 128:(t + 1) * 128], wv[:, kc, :], start=(kc == 0), stop=(kc == 7)),
                                        reads=[BWV, HB[t]], writes=[PPB[i]])
                                for hh in range(4):
                                    c0 = 0 if (h0 + hh) % 2 == 0 else 64
                                    eng = "act" if hh % 2 == 0 else "dve"
                                    if eng == "act":
                                        op("act", lambda e, i=i, t=t, hh=hh, c0=c0: e.copy(va[:, t, hh, c0:c0 + 64], pp[i][:, hh * 64:(hh + 1) * 64]),
                                           reads=[PPB[i]], writes=[B_VA[t]])
                                    else:
                                        op("dve", lambda e, i=i, t=t, hh=hh, c0=c0: e.tensor_copy(va[:, t, hh, c0:c0 + 64], pp[i][:, hh * 64:(hh + 1) * 64]),
                                           reads=[PPB[i]], writes=[B_VA[t]])
                            for u in kq_units(0):
                                u()
                            psc = [ps(es, f"fsc{i}", [128, 512], F32) for i in range(4)]; PSC = bufs(4)
                            pov = [ps(es, f"fo{i}", [128, 512], F32) for i in range(2)]; POV = bufs(2)
                            ptl = [sb(es, f"fpt{i}", [128, 512], BF16) for i in range(4)]; PTL = bufs(4)
                            rc = [sb(es, f"frc{i}", [128, 512], F32) for i in range(2)]; RC = bufs(2)
                            its = []
                            no = 0
                            for hh in range(4):
                                h = h0 + hh
                                for jb in range(4):
                                    io = no % 2; no += 1
                                    nk = 4 * jb + 4
                                    for kci in range(nk):
                                        its.append((hh, h, jb, io, nk, kci))

                            def emit_qk(n):
                                hh, h, jb, io, nk, kci = its[n]
                                r = kci - 4 * jb
                                q0 = max(r, 0) * 128
                                qs = slice(jb * 512 + q0, (jb + 1) * 512)
                                w = 512 - q0
                                i3 = n % 4
                                op("pe", lambda e: e.matmul(
                                    psc[i3][:, 0:w], kT[0:65, hh, kci * 128:(kci + 1) * 128], qT[0:65, hh, qs], start=True, stop=True),
                                    reads=[B_KT[hh], B_QT[hh]], writes=[PSC[i3]])

                            def emit_pv(n):
                                hh, h, jb, io, nk, kci = its[n]
                                r0 = (h % 2) * 64
                                d0 = 64 - r0
                                r = kci - 4 * jb
                                q0 = max(r, 0) * 128
                                w = 512 - q0
                                i3 = n % 4; i4 = n % 4
                                op("act", lambda e: e.activation(
                                    ptl[i4][:, 0:w], psc[i3][:, 0:w], AF.Exp, bias=negc[:, kci, h:h + 1], scale=0.125),
                                    reads=[PSC[i3], B_NC], writes=[PTL[i4]])
                                if r >= 0:
                                    op("pool", lambda e: e.tensor_tensor(out=ptl[i4][:, 0:128], in0=ptl[i4][:, 0:128], in1=causalb, op=ALU.mult),
                                       reads=[PTL[i4], B_CST], writes=[PTL[i4]])
                                op("pe", lambda e: e.matmul(
                                    pov[io][:, q0:512], va[:, kci, hh, :], ptl[i4][:, 0:w], start=(kci == 0), stop=(kci == nk - 1)),
                                    reads=[B_VA[kci], PTL[i4]], writes=[POV[io]])
                                if kci == nk - 1:
                                    cs = slice(jb * 512, (jb + 1) * 512)
                                    op("dve", lambda e: e.reciprocal(rc[io][r0:r0 + 64, :], pov[io][d0:d0 + 64, :]),
                                       reads=[POV[io]], writes=[RC[io]])
                                    op("dve", lambda e: e.tensor_tensor(
                                        mixT[r0:r0 + 64, h // 2, cs], pov[io][r0:r0 + 64, :], rc[io][r0:r0 + 64, :], ALU.mult),
                                        reads=[POV[io], RC[io]], writes=[B_MIX])

                            AHEAD = 2
                            pend_units = []
                            cur_head = -1
                            for n in range(len(its) + AHEAD):
                                if n < len(its):
                                    hh_n = its[n][0]
                                    if hh_n != cur_head:
                                        for u in pend_units:
                                            u()
                                        cur_head = hh_n
                                        pend_units = kq_units(hh_n + 1) if hh_n + 1 < 4 else []
                                        since = 0
                                    emit_qk(n)
                                    since += 1
                                    if pend_units and since % 4 == 0:
                                        pend_units.pop(0)()
                                if n - AHEAD >= 0:
                                    emit_pv(n - AHEAD)
                            sc.emit()
                    with ExitStack() as es:
                        qmT = sb(es, "qmTb", [128, 2, S], BF16); B_QM = Buf()
                        mkT = sb(es, "mkTb", [128, 2, 256], BF16); B_MK = Buf()
                        mv = sb(es, "mvb", [128, 2, 4, 128], BF16); B_MV = Buf()
                        wqm = sb(es, "wqm", [128, 8, 256], BF16); BWQ = Buf()
                        load_w_bf16(wqm[:], bwin_d[:, 768:1024].rearrange("(kc p) f -> p kc f", p=128), BWQ)
                        with ExitStack() as es2:
                            mem_kv(es2, b, bmkv_d, hT_res(es2, "bm"), mkT, mv, B_MK, B_MV, "b")
                            pq = ps(es2, "pqb", [128, 512], F32); PQB = Buf()
                            for m in range(2):
                                for sbk in range(4):
                                    for kc in range(8):
                                        op("pe", lambda e, m=m, sbk=sbk, kc=kc: e.matmul(
                                            pq[:], wqm[:, kc, m * 128:(m + 1) * 128], hT[:, kc, sbk * 512:(sbk + 1) * 512],
                                            start=(kc == 0), stop=(kc == 7)), reads=[HB[4 * sbk + j] for j in range(4)] + [BWQ], writes=[PQB])
                                    op("dve", lambda e, m=m, sbk=sbk: e.tensor_copy(qmT[:, m, sbk * 512:(sbk + 1) * 512], pq[:]),
                                       reads=[PQB], writes=[B_QM])
                            sc.emit()
                        mem_attn(es, qmT, B_QM, mkT, mv, B_MK, B_MV, mixT, 6, B_MIX, "b")
                        sc.emit()
                with ExitStack() as es:
                    kch = [(i, 128, 0) for i in range(8)]
                    outproj_ln_router(es, 1, b, mixT, B_MIX, kch,
                                      lambda wout, BW: load_w_bf16(wout[:, :, :], bwout_d.rearrange("(c p) d -> p c d", p=128), BW), hsrc)
                    sc.emit()

        def experts(l):
            with ExitStack() as es:
                b1r = sb(es, "b1r", [E, 2048], F32); BB = Buf()
                op("sp", lambda e: e.dma_start(out=b1r[:], in_=b1_d[l]), writes=[BB], dma=True)
                pb = ps(es, "pb", [128, 16, E], F32); PBB = Buf()
                for c in range(16):
                    op("pe", lambda e, c=c: e.transpose(pb[:, c, :], b1r[:, c * 128:(c + 1) * 128], cst[0:E, C_IDENT:C_IDENT + E]),
                       reads=[BB, B_CST], writes=[PBB])
                op("dve", lambda e: e.tensor_copy(b1T[:, :, :], pb[:, :, :]), reads=[PBB], writes=[B_B1T])
                op("dve", lambda e: e.tensor_scalar(b1T[:, 8:16, :], b1T[:, 8:16, :], 1.0, None, ALU.add), reads=[B_B1T], writes=[B_B1T])
                sc.emit()
            with ExitStack() as es:
                w1 = [sb(es, f"w1_{i}", [128, 8, 2048], BF16) for i in range(2)]; W1B = bufs(2)
                w2 = [sb(es, f"w2_{i}", [128, 8, D], BF16) for i in range(2)]; W2B = bufs(2)
                b2bc = [sb(es, f"b2bc{i}", [128, D], F32) for i in range(2)]; B2B = bufs(2)
                xs = [sb(es, f"xs{i}", [128, 4, D], BF16) for i in range(2)]; XSB = bufs(2)
                xT = [sb(es, f"xT{i}", [128, 8, BLK], BF16) for i in range(2)]; XTB = bufs(2)
                aT = [sb(es, f"aT{i}", [128, 8, BLK], BF16) for i in range(2)]; ATB = bufs(2)
                gt = [sb(es, f"gt{i}", [128, BLK], F32) for i in range(2)]; GTB = bufs(2)
                sg = [sb(es, f"sg{i}", [128, BLK], F32) for i in range(2)]; SGB = bufs(2)
                lt = [sb(es, f"lt{i}", [128, BLK], F32) for i in range(2)]; LTB = bufs(2)
                ysb = [sb(es, f"ysb{i}", [128, D], F32) for i in range(3)]; YB = bufs(3)
                ptr = [ps(es, f"ptx{i}", [128, 8, 128], BF16) for i in range(2)]; PTRB = bufs(2)
                pgl = [ps(es, f"pgl{i}", [128, 2, 512], F32) for i in range(2)]; PGLB = bufs(2)
                py = [ps(es, f"pye{i}", [128, 512], F32) for i in range(2)]; PYB = bufs(2)
                cnt = {"tr": 0, "gl": 0, "yy": 0, "ys": 0}
                blocks = [(ex, bk) for ex in range(E) for bk in range(NBLK)]

                def load_weights(ex):
                    wi = ex % 2
                    for q in range(4):
                        load_w_bf16(w1[wi][:, 2 * q:2 * q + 2, :],
                                    w1_d[l, ex].rearrange("(kc p) f -> p kc f", p=128)[:, 2 * q:2 * q + 2, :], W1B[wi])

                def load_weights2(ex):
                    wi = ex % 2
                    for q in range(2):
                        load_w_bf16(w2[wi][:, 4 * q:4 * q + 4, :],
                                    w2_d[l, ex].rearrange("(kc p) f -> p kc f", p=128)[:, 4 * q:4 * q + 4, :], W2B[wi])
                    op("sp", lambda e: e.dma_start(out=b2bc[wi][:], in_=b2_d[l, ex:ex + 1, :].to_broadcast([128, D])),
                       writes=[B2B[wi]], dma=True)

                def T_load(n):
                    ex, bk = blocks[n]
                    xi = n % 2
                    s0 = ex * CAP + bk * BLK
                    if bk == 0:
                        load_weights(ex)
                    op("sp", lambda e: e.dma_start(
                        out=xs[xi][:], in_=XS_d[s0:s0 + BLK, :].rearrange("(j p) d -> p j d", p=128)),
                        reads=[B_XS], writes=[XSB[xi]], dma=True)

                def T_j(n, j):
                    xi = n % 2
                    ti = cnt["tr"] % 2
                    cnt["tr"] += 1
                    for kc in range(8):
                        op("pe", lambda e, kc=kc: e.transpose(
                            ptr[ti][:, kc, :], xs[xi][:, j, kc * 128:(kc + 1) * 128], identb), reads=[XSB[xi], B_CST], writes=[PTRB[ti]])
                    op("act", lambda e: e.copy(xT[xi][:, :, j * 128:(j + 1) * 128], ptr[ti][:, :, :]),
                       reads=[PTRB[ti]], writes=[XTB[xi]])

                def M1(n, c):
                    ex, bk = blocks[n]
                    wi = ex % 2
                    xi = n % 2
                    gi = cnt["gl"] % 2
                    cnt["gl"] += 1
                    for half in range(2):
                        col = half * 1024 + c * 128
                        for kc in range(8):
                            op("pe", lambda e, half=half, col=col, kc=kc: e.matmul(
                                pgl[gi][:, half, :], w1[wi][:, kc, col:col + 128], xT[xi][:, kc, :], start=(kc == 0), stop=(kc == 7)),
                                reads=[W1B[wi], XTB[xi]], writes=[PGLB[gi]])
                    op("dve", lambda e: e.tensor_scalar(
                        gt[gi][:], pgl[gi][:, 0, :], b1T[:, c, ex:ex + 1], 7.0, ALU.add, ALU.min), reads=[PGLB[gi], B_B1T], writes=[GTB[gi]])
                    op("act", lambda e: e.activation(sg[gi][:], gt[gi][:], AF.Sigmoid, scale=1.702), reads=[GTB[gi]], writes=[SGB[gi]])
                    op("act", lambda e: e.activation(
                        lt[gi][:], pgl[gi][:, 1, :], AF.Identity, bias=b1T[:, 8 + c, ex:ex + 1], scale=1.0),
                        reads=[PGLB[gi], B_B1T], writes=[LTB[gi]])
                    op("dve", lambda e: e.tensor_scalar(lt[gi][:], lt[gi][:], 8.0, -6.0, ALU.min, ALU.max), reads=[LTB[gi]], writes=[LTB[gi]])
                    op("dve", lambda e: e.tensor_tensor(gt[gi][:], gt[gi][:], sg[gi][:], ALU.mult),
                       reads=[GTB[gi], SGB[gi]], writes=[GTB[gi]])
                    op("dve", lambda e: e.tensor_tensor(aT[xi][:, c, :], gt[gi][:], lt[gi][:], ALU.mult),
                       reads=[GTB[gi], LTB[gi]], writes=[ATB[xi]])

                def M2(n, j):
                    ex, bk = blocks[n]
                    wi = ex % 2
                    xi = n % 2
                    s0 = ex * CAP + bk * BLK
                    yi = cnt["ys"] % 3
                    cnt["ys"] += 1
                    for hf in range(2):
                        pi = cnt["yy"] % 2
                        cnt["yy"] += 1
                        for c in range(8):
                            op("pe", lambda e, pi=pi, c=c, hf=hf: e.matmul(
                                py[pi][:], aT[xi][:, c, j * 128:(j + 1) * 128], w2[wi][:, c, hf * 512:(hf + 1) * 512],
                                start=(c == 0), stop=(c == 7)), reads=[ATB[xi], W2B[wi]], writes=[PYB[pi]])
                        op("dve", lambda e, pi=pi, hf=hf: e.tensor_tensor(
                            ysb[yi][:, hf * 512:(hf + 1) * 512], py[pi][:], b2bc[wi][:, hf * 512:(hf + 1) * 512], ALU.add),
                            reads=[PYB[pi], B2B[wi]], writes=[YB[yi]])
                    op("act", lambda e: e.dma_start(out=YS_d[s0 + j * 128:s0 + (j + 1) * 128, :], in_=ysb[yi][:]),
                       reads=[YB[yi]], writes=[B_YS], dma=True)

                nb_tot = len(blocks)
                T_load(0)
                load_weights2(0)
                for j in range(4):
                    T_j(0, j)
                for n in range(nb_tot):
                    for c in range(8):
                        M1(n, c)
                        if c % 2 == 1:
                            j = c // 2
                            if n >= 1:
                                M2(n - 1, j)
                            if n + 1 < nb_tot:
                                if j == 0:
                                    T_load(n + 1)
                                T_j(n + 1, j)
                    ex_n, bk_n = blocks[n]
                    if bk_n == 0 and ex_n + 1 < E:
                        load_weights2(ex_n + 1)
                for j in range(4):
                    M2(nb_tot - 1, j)
                sc.emit()

        def combine(l):
            with ExitStack() as es:
                g_bc, b_bc, BG = ln_bc(es, ln2g_d, ln2b_d, l, "2")
                set_bc()
                NBUF = 2
                yg = [[sb(es, f"yg{i}_{k}", [128, D], F32) for k in range(4)] for i in range(NBUF)]
                YGB = [bufs(4) for _ in range(NBUF)]
                ht = [sb(es, f"cht{i}", [128, D], F32) for i in range(NBUF)]; HTB = bufs(NBUF)
                acc = [sb(es, f"acc{i}", [128, D], F32) for i in range(NBUF)]; ACB = bufs(NBUF)
                o = [sb(es, f"co{i}", [128, D], F32) for i in range(NBUF)]; OB = bufs(NBUF)
                st = [(sb(es, f"cst{i}", [128, 12], F32), sb(es, f"cmv{i}", [128, 2], F32),
                       sb(es, f"crstd{i}", [128, 1], F32), sb(es, f"cnmr{i}", [128, 1], F32)) for i in range(NBUF)]
                STB = bufs(NBUF)
                for tt in range(NT):
                    i = tt % NBUF
                    for k in range(4):
                        op("pool", lambda e, i=i, tt=tt, k=k: e.indirect_dma_start(
                            out=yg[i][k][:, :], out_offset=None, in_=YS_d[:, :],
                            in_offset=bass.IndirectOffsetOnAxis(ap=idx_all[:, tt, k:k + 1], axis=0),
                            bounds_check=bcreg, oob_is_err=False), reads=[B_YS, B_IDX[tt]], writes=[YGB[i][k]], dma=True)
                    op("sp", lambda e, i=i, tt=tt: e.dma_start(out=ht[i][:], in_=H1_d[tt * 128:(tt + 1) * 128, :]),
                       reads=[B_H1], writes=[HTB[i]], dma=True)
                    op("act", lambda e, i=i, tt=tt: e.activation(acc[i][:], yg[i][0][:], AF.Identity, scale=gate_all[:, tt, 0:1]),
                       reads=[YGB[i][0], B_GATE[tt]], writes=[ACB[i]])
                    for k in range(1, 4):
                        op("dve", lambda e, i=i, tt=tt, k=k: e.scalar_tensor_tensor(
                            out=acc[i][:], in0=yg[i][k][:], scalar=gate_all[:, tt, k:k + 1], in1=acc[i][:], op0=ALU.mult, op1=ALU.add),
                            reads=[YGB[i][k], B_GATE[tt], ACB[i]], writes=[ACB[i]])
                    op("dve", lambda e, i=i: e.scalar_tensor_tensor(
                        out=acc[i][:], in0=ht[i][:], scalar=ALPHA, in1=acc[i][:], op0=ALU.mult, op1=ALU.add),
                        reads=[HTB[i], ACB[i]], writes=[ACB[i]])
                    layer_norm(acc[i], ACB[i], o[i], OB[i], g_bc, b_bc, BG, st[i], STB[i], g_eng="pool")
                    if l == 0:
                        op("sp", lambda e, i=i, tt=tt: e.dma_start(out=H2_d[tt * 128:(tt + 1) * 128, :], in_=o[i][:]),
                           reads=[OB[i]], writes=[B_H2], dma=True)
                    else:
                        bb, tl = divmod(tt, S // 128)
                        op("sp", lambda e, i=i, bb=bb, tl=tl: e.dma_start(out=out_d[bb, tl * 128:(tl + 1) * 128, :], in_=o[i][:]),
                           reads=[OB[i]], writes=[B_OUT], dma=True)
                sc.emit()

        def zero_fill_bg():
            for r0 in range(0, NSLOT, 8192):
                op("act", lambda e, r0=r0: e.dma_start(
                    out=XS_d[r0:r0 + 8192, :].rearrange("(p a j) d -> p a (j d)", p=128, a=16),
                    in_=zt[:].unsqueeze(1).to_broadcast([128, 16, 4096])),
                    writes=[B_XS], dma=True, defer=True)

        def reset_base():
            op("dve", lambda e: e.memset(base_cnt[:], 0.0), writes=[B_BASE])

        def dump_dbg(tag):
            if debug:
                op("sp", lambda e: e.dma_start(out=DBGI_d[:, :], in_=idx_all[:, :, :]), reads=B_IDX, writes=[B_OUT], dma=True)
                op("sp", lambda e: e.dma_start(out=DBGG_d[:, :], in_=gate_all[:, :, :]), reads=B_GATE, writes=[B_OUT], dma=True)
                sc.emit()

        stages = []
        for b in range(NB):
            stages.append(("mixA%d" % b, lambda b=b: mixer_A(b)))
        stages.append(("exp0", lambda: experts(0)))
        stages.append(("comb0", lambda: combine(0)))
        stages.append(("reset", reset_base))
        for b in range(NB):
            stages.append(("mixB%d" % b, lambda b=b: mixer_B(b)))
        stages.append(("exp1", lambda: experts(1)))
        stages.append(("comb1", lambda: combine(1)))
        for name, fn in stages:
            fn()
            if stop_after is not None and name == stop_after:
                break
        dump_dbg("end")
        if any(sc.ops[e] for e in ENGS):
            sc.emit()
    return nc


_CACHE = {}


def _prep_weights(inputs, cap):
    f = lambda a: np.ascontiguousarray(np.asarray(a, dtype=np.float32))
    w1 = np.asarray(inputs["moe_w1"], dtype=np.float32)
    w1p = np.concatenate([w1[..., 0::2], w1[..., 1::2]], axis=-1)
    b1 = np.asarray(inputs["moe_b1"], dtype=np.float32)
    b1p = np.concatenate([b1[..., 0::2], b1[..., 1::2]], axis=-1)
    return {
        "ln1_g": f(inputs["ln1_g"]), "ln1_b": f(inputs["ln1_b"]), "ln2_g": f(inputs["ln2_g"]), "ln2_b": f(inputs["ln2_b"]),
        "a_w_in": f(inputs["a_w_in"][0]), "pool_w": f(inputs["pool_w"][0]), "pool_scale": f(inputs["pool_scale"][0]).reshape(768, 1),
        "a_mem_kv": f(inputs["a_mem_kv"][0]), "a_w_out": f(inputs["a_w_out"][0]),
        "kv_w": f(inputs["kv_w"]), "fgate_b": f(inputs["fgate_b"]).reshape(12, 1),
        "b_w_in": f(inputs["b_w_in"][0]), "b_mem_kv": f(inputs["b_mem_kv"][0]), "b_w_out": f(inputs["b_w_out"][0]),
        "router_w": f(inputs["router_w"]), "router_b": f(inputs["router_b"]),
        "moe_w1": np.ascontiguousarray(w1p), "moe_b1": np.ascontiguousarray(b1p),
        "moe_w2": f(inputs["moe_w2"]), "moe_b2": f(inputs["moe_b2"]),
        "consts": make_consts(cap),
    }


def kernel(**inputs):
    NB, CAP = 4, 1536
    key = (NB, CAP)
    if key not in _CACHE:
        _CACHE[key] = build(NB=NB, CAP=CAP)
    nc = _CACHE[key]
    shared = _prep_weights(inputs, CAP)
    x = np.asarray(inputs["x"], dtype=np.float32)
    mem = np.asarray(inputs["mem"], dtype=np.float32)
    in_maps = []
    for c in range(NCORES):
        m = dict(shared)
        m["x"] = np.ascontiguousarray(x[c * NB:(c + 1) * NB])
        m["mem"] = np.ascontiguousarray(mem[c * NB:(c + 1) * NB])
        in_maps.append(m)
    res = run_bass_kernel_spmd(nc, in_maps, core_ids=list(range(NCORES)))
    return np.concatenate([r["out"] for r in res.results], axis=0).astype(np.float32)
```

```python
import numpy as np
from contextlib import ExitStack
import concourse.bass as bass
import concourse.mybir as mybir
from concourse.bass_utils import run_bass_kernel_spmd

F32 = mybir.dt.float32
BF16 = mybir.dt.bfloat16
I32 = mybir.dt.int32
ALU = mybir.AluOpType
AF = mybir.ActivationFunctionType
AX = mybir.AxisListType

NCORES = 8
S = 2048
D = 1024
E = 32
BLK = 512
ALPHA = float(4 ** 0.25)
EPS = 1e-5
WINS = (2, 4, 8, 16)

ENGS = ("pe", "act", "dve", "pool", "sp")
NDMASEM = 6
SEM_WRAP = 30000


class Buf:
    __slots__ = ("w", "r", "rd")

    def __init__(self):
        self.w = None
        self.r = {}
        self.rd = []


def bufs(n):
    return [Buf() for _ in range(n)]


class Op:
    __slots__ = ("eng", "fn", "deps", "dma", "need", "sem", "val", "phase")

    def __init__(self, eng, fn, dma, phase):
        self.eng = eng
        self.fn = fn
        self.deps = []
        self.dma = dma
        self.need = False
        self.sem = None
        self.val = 0
        self.phase = phase


class Sched:
    def __init__(self, nc):
        self.nc = nc
        self.ops = {e: [] for e in ENGS}
        self.prog = {}
        self.dma_sems = {}
        self.dma_n = {e: 0 for e in ENGS}
        self.nsem = 0
        self.phase = 0
        self.pending_dma = {e: [] for e in ENGS}
        self.deferred = {e: [] for e in ENGS}
        self.nops = 0

    def _newsem(self, tag):
        self.nsem += 1
        return self.nc.alloc_semaphore(f"{tag}_{self.nsem}")

    def collect_deferred(self):
        for e in ENGS:
            self.pending_dma[e].extend(self.deferred[e])
            self.deferred[e] = []

    def op(self, eng, fn, reads=(), writes=(), dma=False, defer=False):
        o = Op(eng, fn, dma, self.phase)
        deps = []
        for b in reads:
            if b.w is not None:
                deps.append(b.w)
        for b in writes:
            if b.w is not None:
                deps.append(b.w)
            deps.extend(b.r.values())
            deps.extend(b.rd)
        seen = set()
        for d in deps:
            if d is o or d.phase != self.phase or id(d) in seen:
                continue
            seen.add(id(d))
            if eng == "pe" and d.eng == "pe" and not d.dma and not dma:
                continue
            o.deps.append(d)
        for b in writes:
            b.w = o
            b.r = {}
            b.rd = []
        for b in reads:
            if dma:
                b.rd.append(o)
            else:
                b.r[eng] = o
        self.ops[eng].append(o)
        if dma:
            (self.deferred if defer else self.pending_dma)[eng].append(o)
        self.nops += 1
        return o

    def emit(self):
        nc = self.nc
        ops = self.ops
        for e in ENGS:
            for o in ops[e]:
                for d in o.deps:
                    d.need = True
                if o.dma:
                    o.need = True
        for e in ENGS:
            for o in ops[e]:
                if o.dma:
                    if e not in self.dma_sems:
                        self.dma_sems[e] = [[self._newsem("d" + e), 0] for _ in range(NDMASEM)]
                    slot = self.dma_sems[e][self.dma_n[e] % NDMASEM]
                    self.dma_n[e] += 1
                    slot[1] += 16
                    o.sem, o.val = slot[0], slot[1]
                elif o.need:
                    p = self.prog.get(e)
                    if p is None or p[1] >= SEM_WRAP:
                        p = [self._newsem("p" + e), 0]
                        self.prog[e] = p
                    p[1] += 1
                    o.sem, o.val = p[0], p[1]
        engmap = {"pe": "tensor", "act": "scalar", "dve": "vector", "pool": "gpsimd", "sp": "sync"}
        with nc.Block() as block:
            for e in ENGS:
                lst = ops[e]
                pend = self.pending_dma[e]

                def body(eng, lst=lst, pend=pend):
                    seen = {}

                    def wait(sem, val):
                        k = sem.num
                        if seen.get(k, 0) >= val:
                            return
                        seen[k] = val
                        eng.wait_ge(sem, val)

                    for o in lst:
                        for d in o.deps:
                            wait(d.sem, d.val)
                        if o.dma and o.val > 16:
                            wait(o.sem, o.val - 16)
                        ins = o.fn(eng)
                        if o.need:
                            ins.then_inc(o.sem, 16 if o.dma else 1)
                    for o in pend:
                        wait(o.sem, o.val)

                if lst or pend:
                    getattr(block, engmap[e])(body)
        self.ops = {e: [] for e in ENGS}
        self.pending_dma = {e: [] for e in ENGS}
        self.phase += 1


C_IDENT = 0
C_LSTRICT = 128
C_ONES = 256
C_BAND = 384
C_CAUSAL = C_BAND + 12 * 128
C_EC = C_CAUSAL + 128
C_SEL = C_EC + 32
NCONST = C_SEL + 12 * 65


def make_consts(cap):
    c = np.zeros((128, NCONST), np.float32)
    i = np.arange(128)
    c[:, C_IDENT:C_IDENT + 128] = np.eye(128, dtype=np.float32)
    c[:, C_LSTRICT:C_LSTRICT + 128] = (i[:, None] < i[None, :]).astype(np.float32)
    c[:, C_ONES:C_ONES + 128] = 1.0
    tp = i[:, None]
    t = i[None, :]
    for g, w in enumerate(WINS):
        diag = ((tp <= t) & (tp > t - w)).astype(np.float32) / w - (tp == t)
        off = ((tp - 128) > (t - w)).astype(np.float32) / w
        cnt = np.minimum(t + 1, w).astype(np.float32)
        first = ((tp <= t) & (tp > t - w)).astype(np.float32) / cnt - (tp == t)
        base = C_BAND + g * 3 * 128
        c[:, base:base + 128] = diag
        c[:, base + 128:base + 256] = off
        c[:, base + 256:base + 384] = first
    c[:, C_CAUSAL:C_CAUSAL + 128] = (tp <= t).astype(np.float32)
    c[:, C_EC:C_EC + 32] = (np.arange(32) * cap)[None, :].astype(np.float32)
    for h in range(12):
        c[h, C_SEL + h * 65 + 64] = 1.0
    return c


def build(NB=4, CAP=1536, debug=False, stop_after=None):
    T = NB * S
    NT = T // 128
    NSLOT = E * CAP
    NBLK = CAP // BLK
    nc = bass.Bass("TRN2", target_bir_lowering=False)
    sc = Sched(nc)

    def dram_in(name, shape, dt=F32):
        return nc.dram_tensor(name, shape, dt, kind="ExternalInput").ap()

    x_d = dram_in("x", [NB, S, D])
    mem_d = dram_in("mem", [NB, 256, D])
    ln1g_d = dram_in("ln1_g", [2, D]); ln1b_d = dram_in("ln1_b", [2, D])
    ln2g_d = dram_in("ln2_g", [2, D]); ln2b_d = dram_in("ln2_b", [2, D])
    awin_d = dram_in("a_w_in", [D, D])
    poolw_d = dram_in("pool_w", [4, 192, 192])
    pscale_d = dram_in("pool_scale", [768, 1])
    amkv_d = dram_in("a_mem_kv", [D, 512])
    awout_d = dram_in("a_w_out", [D, D])
    kvw_d = dram_in("kv_w", [D, 1548])
    fgb_d = dram_in("fgate_b", [12, 1])
    bwin_d = dram_in("b_w_in", [D, D])
    bmkv_d = dram_in("b_mem_kv", [D, 512])
    bwout_d = dram_in("b_w_out", [D, D])
    rw_d = dram_in("router_w", [2, D, E])
    rb_d = dram_in("router_b", [2, E])
    w1_d = dram_in("moe_w1", [2, E, D, 2048])
    b1_d = dram_in("moe_b1", [2, E, 2048])
    w2_d = dram_in("moe_w2", [2, E, D, D])
    b2_d = dram_in("moe_b2", [2, E, D])
    cst_d = dram_in("consts", [128, NCONST])
    out_d = nc.dram_tensor("out", [NB, S, D], F32, kind="ExternalOutput").ap()
    skind = "ExternalOutput" if debug else "Internal"
    H1_d = nc.dram_tensor("H1", [T, D], F32, kind=skind).ap()
    H2_d = nc.dram_tensor("H2", [T, D], F32, kind=skind).ap()
    XS_d = nc.dram_tensor("XS", [NSLOT, D], BF16, kind="Internal").ap()
    YS_d = nc.dram_tensor("YS", [NSLOT, D], F32, kind=skind).ap()
    if debug:
        DBGI_d = nc.dram_tensor("DBGI", [128, NT * 4], I32, kind="ExternalOutput").ap()
        DBGG_d = nc.dram_tensor("DBGG", [128, NT * 4], F32, kind="ExternalOutput").ap()
    B_H1 = Buf(); B_H2 = Buf(); B_XS = Buf(); B_YS = Buf(); B_OUT = Buf()

    uid = [0]

    def sb(es, name, shape, dt):
        uid[0] += 1
        return es.enter_context(nc.sbuf_tensor(f"{name}_{uid[0]}", shape, dt))

    def ps(es, name, shape, dt=F32):
        uid[0] += 1
        return es.enter_context(nc.psum_tensor(f"{name}_{uid[0]}", shape, dt))

    op = sc.op
    bcreg = nc.gpsimd.alloc_register("bcreg")

    def set_bc():
        op("pool", lambda e: e.reg_mov(bcreg, NSLOT - 1))

    with ExitStack() as gs:
        cst = sb(gs, "cst", [128, NCONST], F32)
        cstb = sb(gs, "cstb", [128, C_EC], BF16)
        idx_all = sb(gs, "idx_all", [128, NT, 4], I32)
        gate_all = sb(gs, "gate_all", [128, NT, 4], F32)
        base_cnt = sb(gs, "base_cnt", [128, E], F32)
        b1T = sb(gs, "b1T", [128, 16, E], F32)
        epsc = sb(gs, "epsc", [128, 1], F32)
        B_CST = Buf(); B_IDX = bufs(NT); B_GATE = bufs(NT); B_BASE = Buf(); B_B1T = Buf()

        identf = cst[:, C_IDENT:C_IDENT + 128]
        identb = cstb[:, C_IDENT:C_IDENT + 128]
        onesf = cst[:, C_ONES:C_ONES + 128]
        onesb = cstb[:, C_ONES:C_ONES + 128]
        lstrict = cst[:, C_LSTRICT:C_LSTRICT + 128]
        causalb = cstb[:, C_CAUSAL:C_CAUSAL + 128]

        def band(g, k):
            o = C_BAND + (g * 3 + k) * 128
            return cstb[:, o:o + 128]

        zt = sb(gs, "zt", [128, 4096], BF16)
        BZ = Buf()
        with ExitStack() as es:
            op("sp", lambda e: e.dma_start(out=cst[:], in_=cst_d[:, :]), writes=[B_CST], dma=True)
            op("dve", lambda e: e.tensor_copy(cstb[:], cst[:, 0:C_EC]), reads=[B_CST], writes=[B_CST])
            op("dve", lambda e: e.memset(base_cnt[:], 0.0), writes=[B_BASE])
            op("dve", lambda e: e.memset(epsc[:], EPS), writes=[B_CST])
            op("pool", lambda e: e.memset(zt[:], 0.0), writes=[BZ])
            sc.emit()

        def load_w_bf16(dst, src_ap, wbuf):
            op("pool", lambda e: e.dma_start(out=dst, in_=src_ap), writes=[wbuf], dma=True)

        def hT_res(es, tag):
            xt = [sb(es, f"xt{tag}{i}", [128, D], F32) for i in range(2)]
            ptr = [ps(es, f"ptr{tag}{i}", [128, 8, 128], F32) for i in range(2)]
            return (xt, bufs(2), ptr, bufs(2))

        def make_hT(res, src_rows, hT, HB, nt):
            xt, XB, ptr, PB = res
            for t in range(nt):
                i = t % 2
                op("sp", lambda e, t=t, i=i: e.dma_start(out=xt[i][:], in_=src_rows(t)), writes=[XB[i]], dma=True)
                for kc in range(8):
                    op("pe", lambda e, i=i, kc=kc: e.transpose(ptr[i][:, kc, :], xt[i][:, kc * 128:(kc + 1) * 128], identf),
                       reads=[XB[i], B_CST], writes=[PB[i]])
                eng = "act" if t % 2 == 0 else "dve"
                if eng == "act":
                    op("act", lambda e, t=t, i=i: e.copy(hT[:, :, t * 128:(t + 1) * 128], ptr[i][:, :, :]),
                       reads=[PB[i]], writes=[HB[t]])
                else:
                    op("dve", lambda e, t=t, i=i: e.tensor_copy(hT[:, :, t * 128:(t + 1) * 128], ptr[i][:, :, :]),
                       reads=[PB[i]], writes=[HB[t]])

        def mem_kv(es, b, wkv_d, res, mkT, mv, B_MK, B_MV, tag):
            wkv = sb(es, f"wkv{tag}", [128, 8, 512], BF16); BW = Buf()
            load_w_bf16(wkv[:], wkv_d.rearrange("(kc p) f -> p kc f", p=128), BW)
            memT = sb(es, f"memT{tag}", [128, 8, 256], BF16); MB = bufs(2)
            make_hT(res, lambda t: mem_d[b, t * 128:(t + 1) * 128, :], memT, MB, 2)
            pm = ps(es, f"pm{tag}", [128, 512], F32); PMB = Buf()
            op("pool", lambda e: e.memset(mv[:], 1.0), writes=[B_MV])
            for m in range(2):
                for kc in range(8):
                    op("pe", lambda e, m=m, kc=kc: e.matmul(pm[:, 0:256], wkv[:, kc, m * 128:(m + 1) * 128], memT[:, kc, :],
                                                            start=(kc == 0), stop=(kc == 7)),
                       reads=[BW, MB[0], MB[1]], writes=[PMB])
                op("dve", lambda e, m=m: e.tensor_copy(mkT[:, m, :], pm[:, 0:256]), reads=[PMB], writes=[B_MK])
            for mc in range(2):
                for kc in range(8):
                    op("pe", lambda e, mc=mc, kc=kc: e.matmul(pm[:, 0:256], memT[:, kc, mc * 128:(mc + 1) * 128], wkv[:, kc, 256:512],
                                                              start=(kc == 0), stop=(kc == 7)),
                       reads=[BW, MB[mc]], writes=[PMB])
                for h in range(4):
                    c0 = 0 if h % 2 == 0 else 64
                    op("dve", lambda e, mc=mc, h=h, c0=c0: e.tensor_copy(mv[:, mc, h, c0:c0 + 64], pm[:, h * 64:(h + 1) * 64]),
                       reads=[PMB], writes=[B_MV])

        def mem_attn(es, qmT, B_QM, mkT, mv, B_MK, B_MV, mixT, mix_chunk0, B_MIX, tag):
            psc = [ps(es, f"msc{tag}{i}", [128, 512], F32) for i in range(2)]; PSC = bufs(2)
            po = [ps(es, f"mo{tag}{i}", [128, 512], F32) for i in range(2)]; PO = bufs(2)
            pt = [sb(es, f"mpt{tag}{i}", [128, 512], BF16) for i in range(3)]; PT = bufs(3)
            rc = [sb(es, f"mrc{tag}{i}", [128, 512], F32) for i in range(2)]; RC = bufs(2)
            its = []
            no = 0
            for h in range(4):
                for sbk in range(S // 512):
                    io = no % 2
                    no += 1
                    for mc in range(2):
                        its.append((h, sbk, io, mc))

            def emit_qk(n):
                h, sbk, io, mc = its[n]
                m = h // 2
                r0 = (h % 2) * 64
                cs = slice(sbk * 512, (sbk + 1) * 512)
                i2 = n % 2
                op("pe", lambda e: e.matmul(
                    psc[i2][:], mkT[r0:r0 + 64, m, mc * 128:(mc + 1) * 128], qmT[r0:r0 + 64, m, cs],
                    start=True, stop=True), reads=[B_MK, B_QM], writes=[PSC[i2]])

            def emit_pv(n):
                h, sbk, io, mc = its[n]
                m = h // 2
                r0 = (h % 2) * 64
                d0 = 64 - r0
                cs = slice(sbk * 512, (sbk + 1) * 512)
                i2 = n % 2
                i3 = n % 3
                op("act", lambda e: e.activation(pt[i3][:], psc[i2][:], AF.Exp, scale=0.125),
                   reads=[PSC[i2]], writes=[PT[i3]])
                op("pe", lambda e: e.matmul(
                    po[io][:], mv[:, mc, h, :], pt[i3][:], start=(mc == 0), stop=(mc == 1)),
                    reads=[B_MV, PT[i3]], writes=[PO[io]])
                if mc == 1:
                    op("dve", lambda e: e.reciprocal(rc[io][r0:r0 + 64, :], po[io][d0:d0 + 64, :]),
                       reads=[PO[io]], writes=[RC[io]])
                    op("dve", lambda e: e.tensor_tensor(
                        mixT[r0:r0 + 64, mix_chunk0 + m, cs], po[io][r0:r0 + 64, :], rc[io][r0:r0 + 64, :], ALU.mult),
                        reads=[PO[io], RC[io]], writes=[B_MIX])

            for n in range(len(its) + 1):
                if n < len(its):
                    emit_qk(n)
                if n - 1 >= 0:
                    emit_pv(n - 1)

        def ln_bc(es, g_d, b_d, l, tag):
            g_bc = sb(es, f"g_bc{tag}", [128, D], F32)
            b_bc = sb(es, f"b_bc{tag}", [128, D], F32)
            BG = Buf()
            op("sp", lambda e: e.dma_start(out=g_bc[:], in_=g_d[l:l + 1, :].to_broadcast([128, D])), writes=[BG], dma=True)
            op("sp", lambda e: e.dma_start(out=b_bc[:], in_=b_d[l:l + 1, :].to_broadcast([128, D])), writes=[BG], dma=True)
            return g_bc, b_bc, BG

        def layer_norm(z, ZB, out, OB, g_bc, b_bc, BG, st, STB, g_eng="dve"):
            stats, mv, rstd, nmr = st
            for c in range(2):
                op("dve", lambda e, c=c: e.bn_stats(stats[:, c * 6:(c + 1) * 6], z[:, c * 512:(c + 1) * 512]), reads=[ZB], writes=[STB])
            op("dve", lambda e: e.bn_aggr(mv[:], stats[:]), reads=[STB], writes=[STB])
            op("act", lambda e: e.activation(rstd[:], mv[:, 1:2], AF.Ln, bias=epsc[:, 0:1], scale=1.0), reads=[STB, B_CST], writes=[STB])
            op("act", lambda e: e.activation(rstd[:], rstd[:], AF.Exp, scale=-0.5), reads=[STB], writes=[STB])
            op("dve", lambda e: e.scalar_tensor_tensor(out=nmr[:], in0=mv[:, 0:1], scalar=-1.0, in1=rstd[:], op0=ALU.mult, op1=ALU.mult),
               reads=[STB], writes=[STB])
            op("act", lambda e: e.activation(z[:], z[:], AF.Identity, bias=nmr[:, 0:1], scale=rstd[:, 0:1]), reads=[ZB, STB], writes=[ZB])
            if g_eng == "pool":
                op("pool", lambda e: e.tensor_tensor(out=z[:], in0=z[:], in1=g_bc[:], op=ALU.mult), reads=[ZB, BG], writes=[ZB])
            else:
                op("dve", lambda e: e.tensor_tensor(z[:], z[:], g_bc[:], ALU.mult), reads=[ZB, BG], writes=[ZB])
            op("dve", lambda e: e.tensor_tensor(out[:], z[:], b_bc[:], ALU.add), reads=[ZB, BG], writes=[OB])

        def outproj_ln_router(es, l, b, mixT, B_MIX, kchunks, wout_d, h_rows):
            nk = len(kchunks)
            set_bc()
            wout = sb(es, "wout", [128, nk, D], BF16); BW = Buf()
            wout_d(wout, BW)
            g_bc, b_bc, BG = ln_bc(es, ln1g_d, ln1b_d, l, "1")
            rw = sb(es, "rw", [128, 8, E], F32); BR = Buf()
            op("sp", lambda e: e.dma_start(out=rw[:], in_=rw_d[l].rearrange("(kc p) e -> p kc e", p=128)), writes=[BR], dma=True)
            rb_bc = sb(es, "rb_bc", [128, E], F32)
            op("sp", lambda e: e.dma_start(out=rb_bc[:], in_=rb_d[l:l + 1, :].to_broadcast([128, E])), writes=[BR], dma=True)
            NBUF = 3
            ht = [sb(es, f"ht{i}", [128, D], F32) for i in range(NBUF)]; HTB = bufs(NBUF)
            z = [sb(es, f"z{i}", [128, D], F32) for i in range(NBUF)]; ZB = bufs(NBUF)
            h1 = [sb(es, f"h1{i}", [128, D], F32) for i in range(NBUF)]; H1B = bufs(NBUF)
            h1b = [sb(es, f"h1b{i}", [128, D], BF16) for i in range(NBUF)]; H1BB = bufs(NBUF)
            h1T = [sb(es, f"h1T{i}", [128, 8, 128], F32) for i in range(2)]; H1TB = bufs(2)
            st = [(sb(es, f"st{i}", [128, 12], F32), sb(es, f"mv{i}", [128, 2], F32),
                   sb(es, f"rstd{i}", [128, 1], F32), sb(es, f"nmr{i}", [128, 1], F32)) for i in range(NBUF)]
            STB = bufs(NBUF)
            po = [ps(es, f"po{i}", [128, D], F32) for i in range(2)]; POB = bufs(2)
            ptr = ps(es, "ptrr", [128, 8, 128], F32); PTRB = Buf()
            pr = ps(es, "prt", [128, 512], F32)
            PLGB = bufs(2); PCMB = Buf()
            plg = [pr[:, 0:E], pr[:, 64:64 + E]]
            pcm = pr[:, 128:128 + 2 * E]
            NR = 2
            rt = []
            for i in range(NR):
                rt.append(dict(
                    lg=sb(es, f"lg{i}", [128, E], F32), top8=sb(es, f"top8{i}", [128, 8], F32),
                    ex=sb(es, f"ex{i}", [128, 4], F32), ssum=sb(es, f"ssum{i}", [128, 1], F32), nv0=sb(es, f"nv0{i}", [128, 1], F32),
                    Mm=sb(es, f"Mm{i}", [128, E], F32), sbase=sb(es, f"sbase{i}", [128, E], F32),
                    oh=sb(es, f"oh{i}", [128, 4, E], F32), slotf=sb(es, f"slotf{i}", [128, 4], F32)))
            RBS = bufs(NR)
            NTL = S // 128

            def S1(tl):
                i = tl % NBUF
                ip = tl % 2
                t0 = tl * 128
                for hf in range(2):
                    for k, (ci, rows, ro) in enumerate(kchunks):
                        op("pe", lambda e, hf=hf, k=k, ci=ci, rows=rows: e.matmul(
                            po[ip][:, hf * 512:(hf + 1) * 512], mixT[0:rows, ci, t0:t0 + 128], wout[0:rows, k, hf * 512:(hf + 1) * 512],
                            start=(k == 0), stop=(k == nk - 1)), reads=[B_MIX, BW], writes=[POB[ip]])
                op("sp", lambda e: e.dma_start(out=ht[i][:], in_=h_rows(tl)), writes=[HTB[i]], dma=True)

            def S1b(tl):
                i = tl % NBUF
                ip = tl % 2
                for hf in range(2):
                    op("dve", lambda e, hf=hf: e.scalar_tensor_tensor(
                        out=z[i][:, hf * 512:(hf + 1) * 512], in0=ht[i][:, hf * 512:(hf + 1) * 512], scalar=ALPHA,
                        in1=po[ip][:, hf * 512:(hf + 1) * 512], op0=ALU.mult, op1=ALU.add),
                        reads=[HTB[i], POB[ip]], writes=[ZB[i]])
                stats, mv, rstd, nmr = st[i]
                for c in range(2):
                    op("dve", lambda e, c=c: e.bn_stats(stats[:, c * 6:(c + 1) * 6], z[i][:, c * 512:(c + 1) * 512]), reads=[ZB[i]], writes=[STB[i]])
                op("dve", lambda e: e.bn_aggr(mv[:], stats[:]), reads=[STB[i]], writes=[STB[i]])
                op("act", lambda e: e.activation(rstd[:], mv[:, 1:2], AF.Ln, bias=epsc[:, 0:1], scale=1.0), reads=[STB[i], B_CST], writes=[STB[i]])
                op("act", lambda e: e.activation(rstd[:], rstd[:], AF.Exp, scale=-0.5), reads=[STB[i]], writes=[STB[i]])
                op("dve", lambda e: e.scalar_tensor_tensor(out=nmr[:], in0=mv[:, 0:1], scalar=-1.0, in1=rstd[:], op0=ALU.mult, op1=ALU.mult),
                   reads=[STB[i]], writes=[STB[i]])

            def S2(tl):
                i = tl % NBUF
                i2 = tl % 2
                tt = b * NTL + tl
                stats, mv, rstd, nmr = st[i]
                op("act", lambda e: e.activation(z[i][:], z[i][:], AF.Identity, bias=nmr[:, 0:1], scale=rstd[:, 0:1]), reads=[ZB[i], STB[i]], writes=[ZB[i]])
                op("dve", lambda e: e.tensor_tensor(z[i][:], z[i][:], g_bc[:], ALU.mult), reads=[ZB[i], BG], writes=[ZB[i]])
                op("dve", lambda e: e.tensor_tensor(h1[i][:], z[i][:], b_bc[:], ALU.add), reads=[ZB[i], BG], writes=[H1B[i]])
                op("sp", lambda e: e.dma_start(out=H1_d[tt * 128:(tt + 1) * 128, :], in_=h1[i][:]),
                   reads=[H1B[i]], writes=[B_H1], dma=True)
                op("act", lambda e: e.copy(h1b[i][:], h1[i][:]), reads=[H1B[i]], writes=[H1BB[i]])
                for kc in range(8):
                    op("pe", lambda e, kc=kc: e.transpose(ptr[:, kc, :], h1[i][:, kc * 128:(kc + 1) * 128], identf),
                       reads=[H1B[i], B_CST], writes=[PTRB])
                op("act", lambda e: e.copy(h1T[i2][:, :, :], ptr[:, :, :]), reads=[PTRB], writes=[H1TB[i2]])
                for kc in range(8):
                    op("pe", lambda e, kc=kc: e.matmul(plg[i2], h1T[i2][:, kc, :], rw[:, kc, :], start=(kc == 0), stop=(kc == 7)),
                       reads=[H1TB[i2], BR], writes=[PLGB[i2]])

            def S3(tl):
                i = tl % NBUF
                i2 = tl % 2
                tt = b * NTL + tl
                r = rt[tl % NR]; RB = RBS[tl % NR]
                lg, top8, ex, ssum, nv0, Mm, sbase, oh, slotf = (r[k] for k in ("lg", "top8", "ex", "ssum", "nv0", "Mm", "sbase", "oh", "slotf"))
                op("dve", lambda e: e.tensor_tensor(lg[:], plg[i2], rb_bc[:], ALU.add), reads=[PLGB[i2], BR], writes=[RB])
                op("dve", lambda e: e.max(top8[:], lg[:]), reads=[RB], writes=[RB])
                op("dve", lambda e: e.tensor_scalar(Mm[:], lg[:], top8[:, 3:4], None, ALU.is_ge), reads=[RB], writes=[RB])
                op("pe", lambda e: e.matmul(pcm[:, 0:E], lstrict, Mm[:], start=True, stop=True), reads=[RB, B_CST], writes=[PCMB])
                op("pe", lambda e: e.matmul(pcm[:, E:2 * E], onesf, Mm[:], start=True, stop=True), reads=[RB, B_CST], writes=[PCMB])
                op("dve", lambda e: e.tensor_scalar(nv0[:], top8[:, 0:1], -1.0, None, ALU.mult), reads=[RB], writes=[RB])
                op("act", lambda e: e.activation(ex[:], top8[:, 0:4], AF.Exp, bias=nv0[:, 0:1], scale=1.0), reads=[RB], writes=[RB])
                op("dve", lambda e: e.reduce_sum(ssum[:], ex[:], axis=AX.X), reads=[RB], writes=[RB])
                op("dve", lambda e: e.reciprocal(ssum[:], ssum[:]), reads=[RB], writes=[RB])
                op("dve", lambda e: e.tensor_scalar(gate_all[:, tt, :], ex[:], ssum[:, 0:1], None, ALU.mult),
                   reads=[RB], writes=[B_GATE[tt]])
                op("dve", lambda e: e.tensor_tensor(sbase[:], pcm[:, 0:E], base_cnt[:], ALU.add), reads=[PCMB, B_BASE], writes=[RB])
                op("dve", lambda e: e.tensor_tensor(base_cnt[:], pcm[:, E:2 * E], base_cnt[:], ALU.add), reads=[PCMB, B_BASE], writes=[B_BASE])
                op("dve", lambda e: e.tensor_scalar(sbase[:], sbase[:], float(CAP - 1), None, ALU.min), reads=[RB], writes=[RB])
                op("dve", lambda e: e.tensor_tensor(sbase[:], sbase[:], cst[:, C_EC:C_EC + E], ALU.add), reads=[RB, B_CST], writes=[RB])
                lg_b = lg[:].unsqueeze(1).to_broadcast([128, 4, E])
                tv_b = top8[:, 0:4].unsqueeze(2).to_broadcast([128, 4, E])
                sb_b = sbase[:].unsqueeze(1).to_broadcast([128, 4, E])
                op("dve", lambda e: e.tensor_tensor(oh[:, :, :], lg_b, tv_b, ALU.is_equal), reads=[RB], writes=[RB])
                op("dve", lambda e: e.tensor_tensor(oh[:, :, :], oh[:, :, :], sb_b, ALU.mult), reads=[RB], writes=[RB])
                op("dve", lambda e: e.tensor_reduce(out=slotf[:], in_=oh[:, :, :], axis=AX.X, op=ALU.add), reads=[RB], writes=[RB])
                op("dve", lambda e: e.tensor_copy(idx_all[:, tt, :], slotf[:]), reads=[RB], writes=[B_IDX[tt]])
                for k in range(4):
                    op("pool", lambda e, k=k: e.indirect_dma_start(
                        out=XS_d[:, :], out_offset=bass.IndirectOffsetOnAxis(ap=idx_all[:, tt, k:k + 1], axis=0),
                        in_=h1b[i][:, :], in_offset=None, bounds_check=bcreg, oob_is_err=False),
                        reads=[H1BB[i], B_IDX[tt]], writes=[B_XS], dma=True)

            for n in range(NTL + 2):
                if n < NTL:
                    S1(n)
                if 0 <= n - 2 < NTL:
                    S3(n - 2)
                if 0 <= n - 1 < NTL:
                    S2(n - 1)
                if n < NTL:
                    S1b(n)

        def mixer_A(b):
            with ExitStack() as e0:
                mixT = sb(e0, "mixT", [128, 10, S], BF16); B_MIX = Buf()
                with ExitStack() as e1:
                    u = sb(e1, "u", [128, 16, 768], BF16); UB = bufs(16)
                    qmT = sb(e1, "qmT", [128, 2, S], BF16); B_QM = Buf()
                    mkT = sb(e1, "mkT", [128, 2, 256], BF16); B_MK = Buf()
                    mv = sb(e1, "mv", [128, 2, 4, 128], BF16); B_MV = Buf()
                    with ExitStack() as es:
                        if b == 0:
                            zero_fill_bg()
                        hT = sb(es, "hT", [128, 8, S], BF16); HB = bufs(16)
                        win = sb(es, "win", [128, 8, D], BF16); BW = Buf()
                        load_w_bf16(win[:], awin_d.rearrange("(kc p) f -> p kc f", p=128), BW)
                        res = hT_res(es, "a")
                        make_hT(res, lambda t: x_d[b, t * 128:(t + 1) * 128, :], hT, HB, 16)
                        mem_kv(es, b, amkv_d, res, mkT, mv, B_MK, B_MV, "a")
                        pu = ps(es, "pu", [128, 1024], F32); PUB = Buf()
                        pq = ps(es, "pq", [128, 512], F32); PQB = Buf()
                        for t in range(16):
                            for (c0, c1) in ((0, 512), (512, 768)):
                                for kc in range(8):
                                    op("pe", lambda e, t=t, c0=c0, c1=c1, kc=kc: e.matmul(
                                        pu[:, c0:c1], hT[:, kc, t * 128:(t + 1) * 128], win[:, kc, c0:c1], start=(kc == 0), stop=(kc == 7)),
                                        reads=[HB[t], BW], writes=[PUB])
                            op("act", lambda e, t=t: e.copy(u[:, t, :], pu[:, 0:768]), reads=[PUB], writes=[UB[t]])
                        for m in range(2):
                            for sbk in range(4):
                                for kc in range(8):
                                    op("pe", lambda e, m=m, sbk=sbk, kc=kc: e.matmul(
                                        pq[:], win[:, kc, 768 + m * 128:768 + (m + 1) * 128], hT[:, kc, sbk * 512:(sbk + 1) * 512],
                                        start=(kc == 0), stop=(kc == 7)), reads=[HB[4 * sbk + j] for j in range(4)] + [BW], writes=[PQB])
                                op("dve", lambda e, m=m, sbk=sbk: e.tensor_copy(qmT[:, m, sbk * 512:(sbk + 1) * 512], pq[:]),
                                   reads=[PQB], writes=[B_QM])
                        sc.emit()
                    with ExitStack() as es:
                        dT = sb(es, "dT", [128, 8, S], BF16); DB = Buf()
                        pw = sb(es, "pw", [128, 4, 2, 192], BF16); BPW = Buf()
                        for g in range(4):
                            load_w_bf16(pw[:, g, 0, :], poolw_d[g, 0:128, :], BPW)
                            load_w_bf16(pw[0:64, g, 1, :], poolw_d[g, 128:192, :], BPW)
                        psc_t = sb(es, "psc_t", [128, 8], F32); BPS = Buf()
                        for g in range(4):
                            op("sp", lambda e, g=g: e.dma_start(out=psc_t[:, 2 * g:2 * g + 1], in_=pscale_d[192 * g:192 * g + 128, :]),
                               writes=[BPS], dma=True)
                            op("sp", lambda e, g=g: e.dma_start(out=psc_t[0:64, 2 * g + 1:2 * g + 2], in_=pscale_d[192 * g + 128:192 * g + 192, :]),
                               writes=[BPS], dma=True)
                        pd = [ps(es, f"pd{i}", [128, 512], F32) for i in range(2)]; PDB = bufs(2)
                        n = 0
                        for g in range(4):
                            for ch, (c0, rows) in enumerate(((0, 128), (128, 64))):
                                cc = 192 * g + c0
                                for q4 in range(4):
                                    i = n % 2
                                    n += 1
                                    for j in range(4):
                                        t = q4 * 4 + j
                                        if t == 0:
                                            op("pe", lambda e, i=i, j=j, t=t, cc=cc, rows=rows, g=g: e.matmul(
                                                pd[i][0:rows, j * 128:(j + 1) * 128], u[:, t, cc:cc + rows], band(g, 2), start=True, stop=True),
                                                reads=[UB[t], B_CST], writes=[PDB[i]])
                                        else:
                                            op("pe", lambda e, i=i, j=j, t=t, cc=cc, rows=rows, g=g: e.matmul(
                                                pd[i][0:rows, j * 128:(j + 1) * 128], u[:, t, cc:cc + rows], band(g, 0), start=True, stop=False),
                                                reads=[UB[t], B_CST], writes=[PDB[i]])
                                            op("pe", lambda e, i=i, j=j, t=t, cc=cc, rows=rows, g=g: e.matmul(
                                                pd[i][0:rows, j * 128:(j + 1) * 128], u[:, t - 1, cc:cc + rows], band(g, 1), start=False, stop=True),
                                                reads=[UB[t - 1], B_CST], writes=[PDB[i]])
                                    eng = "act" if n % 2 == 0 else "dve"
                                    if eng == "act":
                                        op("act", lambda e, i=i, rows=rows, g=g, ch=ch, q4=q4: e.copy(
                                            dT[0:rows, 2 * g + ch, q4 * 512:(q4 + 1) * 512], pd[i][0:rows, :]), reads=[PDB[i]], writes=[DB])
                                    else:
                                        op("dve", lambda e, i=i, rows=rows, g=g, ch=ch, q4=q4: e.tensor_copy(
                                            dT[0:rows, 2 * g + ch, q4 * 512:(q4 + 1) * 512], pd[i][0:rows, :]), reads=[PDB[i]], writes=[DB])
                        py = [ps(es, f"py{i}", [128, 512], F32) for i in range(2)]; PYB = bufs(2)
                        n = 0
                        for g in range(4):
                            for och, (o0, orows) in enumerate(((0, 128), (128, 64))):
                                for q4 in range(4):
                                    i = n % 2
                                    n += 1
                                    for ich, irows in enumerate((128, 64)):
                                        op("pe", lambda e, i=i, g=g, o0=o0, orows=orows, ich=ich, irows=irows, q4=q4: e.matmul(
                                            py[i][0:orows, :], pw[0:irows, g, ich, o0:o0 + orows], dT[0:irows, 2 * g + ich, q4 * 512:(q4 + 1) * 512],
                                            start=(ich == 0), stop=(ich == 1)), reads=[BPW, DB], writes=[PYB[i]])
                                    op("act", lambda e, i=i, g=g, och=och, orows=orows, q4=q4: e.activation(
                                        mixT[0:orows, 2 * g + och, q4 * 512:(q4 + 1) * 512], py[i][0:orows, :], AF.Identity,
                                        scale=psc_t[0:orows, 2 * g + och:2 * g + och + 1]), reads=[PYB[i], BPS], writes=[B_MIX])
                        mem_attn(es, qmT, B_QM, mkT, mv, B_MK, B_MV, mixT, 8, B_MIX, "a")
                        if b == 0:
                            sc.collect_deferred()
                        sc.emit()
                with ExitStack() as es:
                    kch = [(2 * g, 128, 0) for g in range(4)] + [(2 * g + 1, 64, 0) for g in range(4)] + [(8, 128, 0), (9, 128, 0)]

                    def load_wout_a(wout, BW):
                        src = awout_d[0:768, :].rearrange("(g r) d -> r g d", r=192)
                        load_w_bf16(wout[:, 0:4, :], src[0:128, :, :], BW)
                        load_w_bf16(wout[0:64, 4:8, :], src[128:192, :, :], BW)
                        load_w_bf16(wout[:, 8:10, :], awout_d[768:1024, :].rearrange("(c p) d -> p c d", p=128), BW)
                    outproj_ln_router(es, 0, b, mixT, B_MIX, kch, load_wout_a, lambda tl: x_d[b, tl * 128:(tl + 1) * 128, :])
                    sc.emit()

        def mixer_B(b):
            hsrc = lambda t: H2_d[b * S + t * 128: b * S + (t + 1) * 128, :]
            with ExitStack() as e0:
                mixT = sb(e0, "mixTb", [128, 8, S], BF16); B_MIX = Buf()
                with ExitStack() as e1:
                    hT = sb(e1, "hTb", [128, 8, S], BF16); HB = bufs(16)
                    negc = sb(e1, "negc", [128, 16, 12], F32); B_NC = Buf()
                    c8T = sb(e1, "c8T", [12, S], BF16); B_C8 = Buf()
                    selb = sb(e1, "selb", [12, 12, 65], BF16); B_SEL = Buf()
                    kT = sb(e1, "kT", [65, 4, S], BF16)
                    qT = sb(e1, "qT", [65, 4, S], BF16)
                    va = sb(e1, "va", [128, 16, 4, 128], BF16)
                    B_ONES = Buf()
                    op("pool", lambda e: e.memset(kT[64:65, :, :], 1.0), writes=[B_ONES])
                    op("dve", lambda e: e.memset(va[:], 1.0), writes=[B_ONES])
                    with ExitStack() as es:
                        make_hT(hT_res(es, "b"), hsrc, hT, HB, 16)
                        wg = sb(es, "wg", [128, 8, 12], BF16); BW = Buf()
                        load_w_bf16(wg[:], kvw_d[:, 1536:1548].rearrange("(kc p) f -> p kc f", p=128), BW)
                        fgb = sb(es, "fgb", [12, 1], F32); BF_ = Buf()
                        op("sp", lambda e: e.dma_start(out=fgb[:], in_=fgb_d[:, :]), writes=[BF_], dma=True)
                        op("dve", lambda e: e.tensor_scalar(fgb[:], fgb[:], -1.0, None, ALU.mult), reads=[BF_], writes=[BF_])
                        op("dve", lambda e: e.tensor_copy(selb[:, :, :], cst[0:12, C_SEL:C_SEL + 12 * 65]), reads=[B_CST], writes=[B_SEL])
                        sp_t = sb(es, "sp_t", [12, S], F32); BSP = Buf()
                        cT = sb(es, "cT", [12, S], F32); BCT = Buf()
                        ones12 = sb(es, "ones12", [12, S], F32); BO = Buf()
                        op("pool", lambda e: e.memset(ones12[:], 1.0), writes=[BO])
                        pg = [ps(es, f"pg{i}", [128, 512], F32) for i in range(2)]; PGB = bufs(2)
                        for sbk in range(4):
                            i = sbk % 2
                            for kc in range(8):
                                op("pe", lambda e, i=i, sbk=sbk, kc=kc: e.matmul(
                                    pg[i][0:12, :], wg[:, kc, :], hT[:, kc, sbk * 512:(sbk + 1) * 512], start=(kc == 0), stop=(kc == 7)),
                                    reads=[BW] + [HB[4 * sbk + j] for j in range(4)], writes=[PGB[i]])
                            op("act", lambda e, i=i, sbk=sbk: e.activation(sp_t[:, sbk * 512:(sbk + 1) * 512], pg[i][0:12, :], AF.Exp,
                                                                           bias=fgb[:, 0:1], scale=-1.0), reads=[PGB[i], BF_], writes=[BSP])
                        op("act", lambda e: e.activation(sp_t[:], sp_t[:], AF.Ln, bias=1.0, scale=1.0), reads=[BSP], writes=[BSP])
                        op("dve", lambda e: e.tensor_tensor_scan(cT[:], ones12[:], sp_t[:], 0.0, ALU.mult, ALU.add), reads=[BSP, BO], writes=[BCT])
                        op("dve", lambda e: e.tensor_scalar(c8T[:], cT[:], -8.0, None, ALU.mult), reads=[BCT], writes=[B_C8])
                        pt = ps(es, "ptc", [128, 16, 12], F32); PTB = Buf()
                        for ch in range(16):
                            op("pe", lambda e, ch=ch: e.transpose(pt[:, ch, :], cT[:, ch * 128:(ch + 1) * 128], cst[0:12, C_IDENT:C_IDENT + 12]),
                               reads=[BCT, B_CST], writes=[PTB])
                        op("dve", lambda e: e.tensor_copy(negc[:, :, :], pt[:, :, :]), reads=[PTB], writes=[B_NC])
                        sc.emit()
                    for grp in range(3):
                        with ExitStack() as es:
                            h0 = grp * 4
                            B_VA = bufs(16)
                            wk = sb(es, "wk", [128, 8, 256], BF16); BWK = Buf()
                            wv = sb(es, "wv", [128, 8, 256], BF16); BWV = Buf()
                            wq = sb(es, "wq", [128, 8, 4, 65], BF16); BWQ = Buf()
                            load_w_bf16(wk[:], kvw_d[:, h0 * 64:(h0 + 4) * 64].rearrange("(kc p) f -> p kc f", p=128), BWK)
                            load_w_bf16(wv[:], kvw_d[:, 768 + h0 * 64:768 + (h0 + 4) * 64].rearrange("(kc p) f -> p kc f", p=128), BWV)
                            op("pool", lambda e: e.memset(wq[:], 0.0), writes=[BWQ])
                            for hh in range(4):
                                load_w_bf16(wq[:, :, hh, 0:64],
                                            bwin_d[:, (h0 + hh) * 64:(h0 + hh + 1) * 64].rearrange("(kc p) f -> p kc f", p=128), BWQ)
                            B_KT = bufs(4); B_QT = bufs(4)
                            pp = [ps(es, f"pp{i}", [128, 512], F32) for i in range(2)]; PPB = bufs(2)
                            pcount = [0]

                            def kq_units(hh):
                                units = []
                                for sbk in range(4):
                                    cs = slice(sbk * 512, (sbk + 1) * 512)
                                    hbs = [HB[4 * sbk + j] for j in range(4)]

                                    def uk(cs=cs, hbs=hbs):
                                        i = pcount[0] % 2; pcount[0] += 1
                                        for kc in range(8):
                                            op("pe", lambda e, kc=kc: e.matmul(
                                                pp[i][0:64, :], wk[:, kc, hh * 64:(hh + 1) * 64], hT[:, kc, cs], start=(kc == 0), stop=(kc == 7)),
                                                reads=[BWK] + hbs, writes=[PPB[i]])
                                        op("dve", lambda e: e.tensor_copy(kT[0:64, hh, cs], pp[i][0:64, :]), reads=[PPB[i]], writes=[B_KT[hh]])

                                    def uq(cs=cs, hbs=hbs):
                                        i = pcount[0] % 2; pcount[0] += 1
                                        for kc in range(8):
                                            op("pe", lambda e, kc=kc: e.matmul(
                                                pp[i][0:65, :], wq[:, kc, hh, :], hT[:, kc, cs], start=(kc == 0), stop=False),
                                                reads=[BWQ] + hbs, writes=[PPB[i]])
                                        op("pe", lambda e: e.matmul(
                                            pp[i][0:65, :], selb[:, h0 + hh, :], c8T[:, cs], start=False, stop=True),
                                            reads=[B_SEL, B_C8], writes=[PPB[i]])
                                        op("dve", lambda e: e.tensor_copy(qT[0:65, hh, cs], pp[i][0:65, :]), reads=[PPB[i]], writes=[B_QT[hh]])
                                    units.append(uk)
                                    units.append(uq)
                                return units

                            for t in range(16):
                                i = pcount[0] % 2; pcount[0] += 1
                                for kc in range(8):
                                    op("pe", lambda e, i=i, t=t, kc=kc: e.matmul(
                                        pp[i][:, 0:256], hT[:, kc, t * 128:(t + 1) * 128], wv[:, kc, :], start=(kc == 0), stop=(kc == 7)),
                                        reads=[BWV, HB[t]], writes=[PPB[i]])
                                for hh in range(4):
                                    c0 = 0 if (h0 + hh) % 2 == 0 else 64
                                    eng = "act" if hh % 2 == 0 else "dve"
                                    if eng == "act":
                                        op("act", lambda e, i=i, t=t, hh=hh, c0=c0: e.copy(va[:, t, hh, c0:c0 + 64], pp[i][:, hh * 64:(hh + 1) * 64]),
                                           reads=[PPB[i]], writes=[B_VA[t]])
                                    else:
                                        op("dve", lambda e, i=i, t=t, hh=hh, c0=c0: e.tensor_copy(va[:, t, hh, c0:c0 + 64], pp[i][:, hh * 64:(hh + 1) * 64]),
                                           reads=[PPB[i]], writes=[B_VA[t]])
                            for u in kq_units(0):
                                u()
                            psc = [ps(es, f"fsc{i}", [128, 512], F32) for i in range(4)]; PSC = bufs(4)
                            pov = [ps(es, f"fo{i}", [128, 512], F32) for i in range(2)]; POV = bufs(2)
                            ptl = [sb(es, f"fpt{i}", [128, 512], BF16) for i in range(4)]; PTL = bufs(4)
                            rc = [sb(es, f"frc{i}", [128, 512], F32) for i in range(2)]; RC = bufs(2)
                            its = []
                            no = 0
                            for hh in range(4):
                                h = h0 + hh
                                for jb in range(4):
                                    io = no % 2; no += 1
                                    nk = 4 * jb + 4
                                    for kci in range(nk):
                                        its.append((hh, h, jb, io, nk, kci))

                            def emit_qk(n):
                                hh, h, jb, io, nk, kci = its[n]
                                r = kci - 4 * jb
                                q0 = max(r, 0) * 128
                                qs = slice(jb * 512 + q0, (jb + 1) * 512)
                                w = 512 - q0
                                i3 = n % 4
                                op("pe", lambda e: e.matmul(
                                    psc[i3][:, 0:w], kT[0:65, hh, kci * 128:(kci + 1) * 128], qT[0:65, hh, qs], start=True, stop=True),
                                    reads=[B_KT[hh], B_QT[hh]], writes=[PSC[i3]])

                            def emit_pv(n):
                                hh, h, jb, io, nk, kci = its[n]
                                r0 = (h % 2) * 64
                                d0 = 64 - r0
                                r = kci - 4 * jb
                                q0 = max(r, 0) * 128
                                w = 512 - q0
                                i3 = n % 4; i4 = n % 4
                                op("act", lambda e: e.activation(
                                    ptl[i4][:, 0:w], psc[i3][:, 0:w], AF.Exp, bias=negc[:, kci, h:h + 1], scale=0.125),
                                    reads=[PSC[i3], B_NC], writes=[PTL[i4]])
                                if r >= 0:
                                    op("pool", lambda e: e.tensor_tensor(out=ptl[i4][:, 0:128], in0=ptl[i4][:, 0:128], in1=causalb, op=ALU.mult),
                                       reads=[PTL[i4], B_CST], writes=[PTL[i4]])
                                op("pe", lambda e: e.matmul(
                                    pov[io][:, q0:512], va[:, kci, hh, :], ptl[i4][:, 0:w], start=(kci == 0), stop=(kci == nk - 1)),
                                    reads=[B_VA[kci], PTL[i4]], writes=[POV[io]])
                                if kci == nk - 1:
                                    cs = slice(jb * 512, (jb + 1) * 512)
                                    op("dve", lambda e: e.reciprocal(rc[io][r0:r0 + 64, :], pov[io][d0:d0 + 64, :]),
                                       reads=[POV[io]], writes=[RC[io]])
                                    op("dve", lambda e: e.tensor_tensor(
                                        mixT[r0:r0 + 64, h // 2, cs], pov[io][r0:r0 + 64, :], rc[io][r0:r0 + 64, :], ALU.mult),
                                        reads=[POV[io], RC[io]], writes=[B_MIX])

                            AHEAD = 3
                            pend_units = []
                            cur_head = -1
                            for n in range(len(its) + AHEAD):
                                if n < len(its):
                                    hh_n = its[n][0]
                                    if hh_n != cur_head:
                                        for u in pend_units:
                                            u()
                                        cur_head = hh_n
                                        pend_units = kq_units(hh_n + 1) if hh_n + 1 < 4 else []
                                        since = 0
                                    emit_qk(n)
                                    since += 1
                                    if pend_units and since % 4 == 0:
                                        pend_units.pop(0)()
                                if n - AHEAD >= 0:
                                    emit_pv(n - AHEAD)
                            sc.emit()
                    with ExitStack() as es:
                        qmT = sb(es, "qmTb", [128, 2, S], BF16); B_QM = Buf()
                        mkT = sb(es, "mkTb", [128, 2, 256], BF16); B_MK = Buf()
                        mv = sb(es, "mvb", [128, 2, 4, 128], BF16); B_MV = Buf()
                        wqm = sb(es, "wqm", [128, 8, 256], BF16); BWQ = Buf()
                        load_w_bf16(wqm[:], bwin_d[:, 768:1024].rearrange("(kc p) f -> p kc f", p=128), BWQ)
                        with ExitStack() as es2:
                            mem_kv(es2, b, bmkv_d, hT_res(es2, "bm"), mkT, mv, B_MK, B_MV, "b")
                            pq = ps(es2, "pqb", [128, 512], F32); PQB = Buf()
                            for m in range(2):
                                for sbk in range(4):
                                    for kc in range(8):
                                        op("pe", lambda e, m=m, sbk=sbk, kc=kc: e.matmul(
                                            pq[:], wqm[:, kc, m * 128:(m + 1) * 128], hT[:, kc, sbk * 512:(sbk + 1) * 512],
                                            start=(kc == 0), stop=(kc == 7)), reads=[HB[4 * sbk + j] for j in range(4)] + [BWQ], writes=[PQB])
                                    op("dve", lambda e, m=m, sbk=sbk: e.tensor_copy(qmT[:, m, sbk * 512:(sbk + 1) * 512], pq[:]),
                                       reads=[PQB], writes=[B_QM])
                            sc.emit()
                        mem_attn(es, qmT, B_QM, mkT, mv, B_MK, B_MV, mixT, 6, B_MIX, "b")
                        sc.emit()
                with ExitStack() as es:
                    kch = [(i, 128, 0) for i in range(8)]
                    outproj_ln_router(es, 1, b, mixT, B_MIX, kch,
                                      lambda wout, BW: load_w_bf16(wout[:, :, :], bwout_d.rearrange("(c p) d -> p c d", p=128), BW), hsrc)
                    sc.emit()

        def experts(l):
            with ExitStack() as es:
                b1r = sb(es, "b1r", [E, 2048], F32); BB = Buf()
                op("sp", lambda e: e.dma_start(out=b1r[:], in_=b1_d[l]), writes=[BB], dma=True)
                pb = ps(es, "pb", [128, 16, E], F32); PBB = Buf()
                for c in range(16):
                    op("pe", lambda e, c=c: e.transpose(pb[:, c, :], b1r[:, c * 128:(c + 1) * 128], cst[0:E, C_IDENT:C_IDENT + E]),
                       reads=[BB, B_CST], writes=[PBB])
                op("dve", lambda e: e.tensor_copy(b1T[:, :, :], pb[:, :, :]), reads=[PBB], writes=[B_B1T])
                op("dve", lambda e: e.tensor_scalar(b1T[:, 8:16, :], b1T[:, 8:16, :], 1.0, None, ALU.add), reads=[B_B1T], writes=[B_B1T])
                sc.emit()
            with ExitStack() as es:
                w1 = [sb(es, f"w1_{i}", [128, 8, 2048], BF16) for i in range(2)]; W1B = bufs(2)
                w2 = [sb(es, f"w2_{i}", [128, 8, D], BF16) for i in range(2)]; W2B = bufs(2)
                b2bc = [sb(es, f"b2bc{i}", [128, D], F32) for i in range(2)]; B2B = bufs(2)
                xs = [sb(es, f"xs{i}", [128, 4, D], BF16) for i in range(2)]; XSB = bufs(2)
                xT = [sb(es, f"xT{i}", [128, 8, BLK], BF16) for i in range(2)]; XTB = bufs(2)
                aT = [sb(es, f"aT{i}", [128, 8, BLK], BF16) for i in range(2)]; ATB = bufs(2)
                gt = [sb(es, f"gt{i}", [128, BLK], F32) for i in range(2)]; GTB = bufs(2)
                sg = [sb(es, f"sg{i}", [128, BLK], F32) for i in range(2)]; SGB = bufs(2)
                lt = [sb(es, f"lt{i}", [128, BLK], F32) for i in range(2)]; LTB = bufs(2)
                ysb = [sb(es, f"ysb{i}", [128, D], F32) for i in range(3)]; YB = bufs(3)
                ptr = [ps(es, f"ptx{i}", [128, 8, 128], BF16) for i in range(2)]; PTRB = bufs(2)
                pgl = [ps(es, f"pgl{i}", [128, 2, 512], F32) for i in range(2)]; PGLB = bufs(2)
                py = [ps(es, f"pye{i}", [128, 512], F32) for i in range(2)]; PYB = bufs(2)
                cnt = {"tr": 0, "gl": 0, "yy": 0, "ys": 0}
                blocks = [(ex, bk) for ex in range(E) for bk in range(NBLK)]

                def load_weights(ex):
                    wi = ex % 2
                    for q in range(4):
                        load_w_bf16(w1[wi][:, 2 * q:2 * q + 2, :],
                                    w1_d[l, ex].rearrange("(kc p) f -> p kc f", p=128)[:, 2 * q:2 * q + 2, :], W1B[wi])

                def load_weights2(ex):
                    wi = ex % 2
                    for q in range(2):
                        load_w_bf16(w2[wi][:, 4 * q:4 * q + 4, :],
                                    w2_d[l, ex].rearrange("(kc p) f -> p kc f", p=128)[:, 4 * q:4 * q + 4, :], W2B[wi])
                    op("sp", lambda e: e.dma_start(out=b2bc[wi][:], in_=b2_d[l, ex:ex + 1, :].to_broadcast([128, D])),
                       writes=[B2B[wi]], dma=True)

                def T_load(n):
                    ex, bk = blocks[n]
                    xi = n % 2
                    s0 = ex * CAP + bk * BLK
                    if bk == 0:
                        load_weights(ex)
                    op("sp", lambda e: e.dma_start(
                        out=xs[xi][:], in_=XS_d[s0:s0 + BLK, :].rearrange("(j p) d -> p j d", p=128)),
                        reads=[B_XS], writes=[XSB[xi]], dma=True)

                def T_j(n, j):
                    xi = n % 2
                    ti = cnt["tr"] % 2
                    cnt["tr"] += 1
                    for kc in range(8):
                        op("pe", lambda e, kc=kc: e.transpose(
                            ptr[ti][:, kc, :], xs[xi][:, j, kc * 128:(kc + 1) * 128], identb), reads=[XSB[xi], B_CST], writes=[PTRB[ti]])
                    op("act", lambda e: e.copy(xT[xi][:, :, j * 128:(j + 1) * 128], ptr[ti][:, :, :]),
                       reads=[PTRB[ti]], writes=[XTB[xi]])

                def M1(n, c):
                    ex, bk = blocks[n]
                    wi = ex % 2
                    xi = n % 2
                    gi = cnt["gl"] % 2
                    cnt["gl"] += 1
                    for half in range(2):
                        col = half * 1024 + c * 128
                        for kc in range(8):
                            op("pe", lambda e, half=half, col=col, kc=kc: e.matmul(
                                pgl[gi][:, half, :], w1[wi][:, kc, col:col + 128], xT[xi][:, kc, :], start=(kc == 0), stop=(kc == 7)),
                                reads=[W1B[wi], XTB[xi]], writes=[PGLB[gi]])
                    op("dve", lambda e: e.tensor_scalar(
                        gt[gi][:], pgl[gi][:, 0, :], b1T[:, c, ex:ex + 1], 7.0, ALU.add, ALU.min), reads=[PGLB[gi], B_B1T], writes=[GTB[gi]])
                    op("act", lambda e: e.activation(sg[gi][:], gt[gi][:], AF.Sigmoid, scale=1.702), reads=[GTB[gi]], writes=[SGB[gi]])
                    op("act", lambda e: e.activation(
                        lt[gi][:], pgl[gi][:, 1, :], AF.Identity, bias=b1T[:, 8 + c, ex:ex + 1], scale=1.0),
                        reads=[PGLB[gi], B_B1T], writes=[LTB[gi]])
                    op("dve", lambda e: e.tensor_scalar(lt[gi][:], lt[gi][:], 8.0, -6.0, ALU.min, ALU.max), reads=[LTB[gi]], writes=[LTB[gi]])
                    op("dve", lambda e: e.tensor_tensor(gt[gi][:], gt[gi][:], sg[gi][:], ALU.mult),
                       reads=[GTB[gi], SGB[gi]], writes=[GTB[gi]])
                    op("dve", lambda e: e.tensor_tensor(aT[xi][:, c, :], gt[gi][:], lt[gi][:], ALU.mult),
                       reads=[GTB[gi], LTB[gi]], writes=[ATB[xi]])

                def M2(n, j):
                    ex, bk = blocks[n]
                    wi = ex % 2
                    xi = n % 2
                    s0 = ex * CAP + bk * BLK
                    yi = cnt["ys"] % 3
                    cnt["ys"] += 1
                    for hf in range(2):
                        pi = cnt["yy"] % 2
                        cnt["yy"] += 1
                        for c in range(8):
                            op("pe", lambda e, pi=pi, c=c, hf=hf: e.matmul(
                                py[pi][:], aT[xi][:, c, j * 128:(j + 1) * 128], w2[wi][:, c, hf * 512:(hf + 1) * 512],
                                start=(c == 0), stop=(c == 7)), reads=[ATB[xi], W2B[wi]], writes=[PYB[pi]])
                        op("dve", lambda e, pi=pi, hf=hf: e.tensor_tensor(
                            ysb[yi][:, hf * 512:(hf + 1) * 512], py[pi][:], b2bc[wi][:, hf * 512:(hf + 1) * 512], ALU.add),
                            reads=[PYB[pi], B2B[wi]], writes=[YB[yi]])
                    op("act", lambda e: e.dma_start(out=YS_d[s0 + j * 128:s0 + (j + 1) * 128, :], in_=ysb[yi][:]),
                       reads=[YB[yi]], writes=[B_YS], dma=True)

                nb_tot = len(blocks)
                T_load(0)
                load_weights2(0)
                for j in range(4):
                    T_j(0, j)
                for n in range(nb_tot):
                    for c in range(8):
                        M1(n, c)
                        if c % 2 == 1:
                            j = c // 2
                            if n >= 1:
                                M2(n - 1, j)
                            if n + 1 < nb_tot:
                                if j == 0:
                                    T_load(n + 1)
                                T_j(n + 1, j)
                    ex_n, bk_n = blocks[n]
                    if bk_n == 0 and ex_n + 1 < E:
                        load_weights2(ex_n + 1)
                for j in range(4):
                    M2(nb_tot - 1, j)
                sc.emit()

        def combine(l):
            with ExitStack() as es:
                g_bc, b_bc, BG = ln_bc(es, ln2g_d, ln2b_d, l, "2")
                set_bc()
                NBUF = 2
                yg = [[sb(es, f"yg{i}_{k}", [128, D], F32) for k in range(4)] for i in range(NBUF)]
                YGB = [bufs(4) for _ in range(NBUF)]
                ht = [sb(es, f"cht{i}", [128, D], F32) for i in range(NBUF)]; HTB = bufs(NBUF)
                acc = [sb(es, f"acc{i}", [128, D], F32) for i in range(NBUF)]; ACB = bufs(NBUF)
                o = [sb(es, f"co{i}", [128, D], F32) for i in range(NBUF)]; OB = bufs(NBUF)
                st = [(sb(es, f"cst{i}", [128, 12], F32), sb(es, f"cmv{i}", [128, 2], F32),
                       sb(es, f"crstd{i}", [128, 1], F32), sb(es, f"cnmr{i}", [128, 1], F32)) for i in range(NBUF)]
                STB = bufs(NBUF)
                for tt in range(NT):
                    i = tt % NBUF
                    for k in range(4):
                        op("pool", lambda e, i=i, tt=tt, k=k: e.indirect_dma_start(
                            out=yg[i][k][:, :], out_offset=None, in_=YS_d[:, :],
                            in_offset=bass.IndirectOffsetOnAxis(ap=idx_all[:, tt, k:k + 1], axis=0),
                            bounds_check=bcreg, oob_is_err=False), reads=[B_YS, B_IDX[tt]], writes=[YGB[i][k]], dma=True)
                    op("sp", lambda e, i=i, tt=tt: e.dma_start(out=ht[i][:], in_=H1_d[tt * 128:(tt + 1) * 128, :]),
                       reads=[B_H1], writes=[HTB[i]], dma=True)
                    op("act", lambda e, i=i, tt=tt: e.activation(acc[i][:], yg[i][0][:], AF.Identity, scale=gate_all[:, tt, 0:1]),
                       reads=[YGB[i][0], B_GATE[tt]], writes=[ACB[i]])
                    for k in range(1, 4):
                        op("dve", lambda e, i=i, tt=tt, k=k: e.scalar_tensor_tensor(
                            out=acc[i][:], in0=yg[i][k][:], scalar=gate_all[:, tt, k:k + 1], in1=acc[i][:], op0=ALU.mult, op1=ALU.add),
                            reads=[YGB[i][k], B_GATE[tt], ACB[i]], writes=[ACB[i]])
                    op("dve", lambda e, i=i: e.scalar_tensor_tensor(
                        out=acc[i][:], in0=ht[i][:], scalar=ALPHA, in1=acc[i][:], op0=ALU.mult, op1=ALU.add),
                        reads=[HTB[i], ACB[i]], writes=[ACB[i]])
                    layer_norm(acc[i], ACB[i], o[i], OB[i], g_bc, b_bc, BG, st[i], STB[i])
                    if l == 0:
                        op("sp", lambda e, i=i, tt=tt: e.dma_start(out=H2_d[tt * 128:(tt + 1) * 128, :], in_=o[i][:]),
                           reads=[OB[i]], writes=[B_H2], dma=True)
                    else:
                        bb, tl = divmod(tt, S // 128)
                        op("sp", lambda e, i=i, bb=bb, tl=tl: e.dma_start(out=out_d[bb, tl * 128:(tl + 1) * 128, :], in_=o[i][:]),
                           reads=[OB[i]], writes=[B_OUT], dma=True)
                sc.emit()

        def zero_fill_bg():
            for r0 in range(0, NSLOT, 8192):
                op("act", lambda e, r0=r0: e.dma_start(
                    out=XS_d[r0:r0 + 8192, :].rearrange("(p a j) d -> p a (j d)", p=128, a=16),
                    in_=zt[:].unsqueeze(1).to_broadcast([128, 16, 4096])),
                    writes=[B_XS], dma=True, defer=True)

        def reset_base():
            op("dve", lambda e: e.memset(base_cnt[:], 0.0), writes=[B_BASE])

        def dump_dbg(tag):
            if debug:
                op("sp", lambda e: e.dma_start(out=DBGI_d[:, :], in_=idx_all[:, :, :]), reads=B_IDX, writes=[B_OUT], dma=True)
                op("sp", lambda e: e.dma_start(out=DBGG_d[:, :], in_=gate_all[:, :, :]), reads=B_GATE, writes=[B_OUT], dma=True)
                sc.emit()

        stages = []
        for b in range(NB):
            stages.append(("mixA%d" % b, lambda b=b: mixer_A(b)))
        stages.append(("exp0", lambda: experts(0)))
        stages.append(("comb0", lambda: combine(0)))
        stages.append(("reset", reset_base))
        for b in range(NB):
            stages.append(("mixB%d" % b, lambda b=b: mixer_B(b)))
        stages.append(("exp1", lambda: experts(1)))
        stages.append(("comb1", lambda: combine(1)))
        for name, fn in stages:
            fn()
            if stop_after is not None and name == stop_after:
                break
        dump_dbg("end")
        if any(sc.ops[e] for e in ENGS):
            sc.emit()
    return nc


_CACHE = {}


def _prep_weights(inputs, cap):
    f = lambda a: np.ascontiguousarray(np.asarray(a, dtype=np.float32))
    w1 = np.asarray(inputs["moe_w1"], dtype=np.float32)
    w1p = np.concatenate([w1[..., 0::2], w1[..., 1::2]], axis=-1)
    b1 = np.asarray(inputs["moe_b1"], dtype=np.float32)
    b1p = np.concatenate([b1[..., 0::2], b1[..., 1::2]], axis=-1)
    return {
        "ln1_g": f(inputs["ln1_g"]), "ln1_b": f(inputs["ln1_b"]), "ln2_g": f(inputs["ln2_g"]), "ln2_b": f(inputs["ln2_b"]),
        "a_w_in": f(inputs["a_w_in"][0]), "pool_w": f(inputs["pool_w"][0]), "pool_scale": f(inputs["pool_scale"][0]).reshape(768, 1),
        "a_mem_kv": f(inputs["a_mem_kv"][0]), "a_w_out": f(inputs["a_w_out"][0]),
        "kv_w": f(inputs["kv_w"]), "fgate_b": f(inputs["fgate_b"]).reshape(12, 1),
        "b_w_in": f(inputs["b_w_in"][0]), "b_mem_kv": f(inputs["b_mem_kv"][0]), "b_w_out": f(inputs["b_w_out"][0]),
        "router_w": f(inputs["router_w"]), "router_b": f(inputs["router_b"]),
        "moe_w1": np.ascontiguousarray(w1p), "moe_b1": np.ascontiguousarray(b1p),
        "moe_w2": f(inputs["moe_w2"]), "moe_b2": f(inputs["moe_b2"]),
        "consts": make_consts(cap),
    }


def kernel(**inputs):
    NB, CAP = 4, 1536
    key = (NB, CAP)
    if key not in _CACHE:
        _CACHE[key] = build(NB=NB, CAP=CAP)
    nc = _CACHE[key]
    shared = _prep_weights(inputs, CAP)
    x = np.asarray(inputs["x"], dtype=np.float32)
    mem = np.asarray(inputs["mem"], dtype=np.float32)
    in_maps = []
    for c in range(NCORES):
        m = dict(shared)
        m["x"] = np.ascontiguousarray(x[c * NB:(c + 1) * NB])
        m["mem"] = np.ascontiguousarray(mem[c * NB:(c + 1) * NB])
        in_maps.append(m)
    res = run_bass_kernel_spmd(nc, in_maps, core_ids=list(range(NCORES)))
    return np.concatenate([r["out"] for r in res.results], axis=0).astype(np.float32)
```
